# Optimizing a Trainium2 kernel written in Bass

```python
import math
import jax, jax.numpy as jnp
from jax import lax
import numpy as np

D_MODEL = 1024
BATCH = 2
SEQ = 8192
DEPTH = 1

EPS = 1e-6
D_SSM = D_MODEL // 2
SSM_GROUP = 16
N_SSM_GROUPS = D_SSM // SSM_GROUP
SSM_STATE = 64
DT_MIN = 1e-3
DT_MAX = 1e-1
HEAD_DIM = 64
N_HEADS = (D_MODEL // 2) // HEAD_DIM
N_KV_HEADS = 2
Q_PER_KV = N_HEADS // N_KV_HEADS
D_ATTN = N_HEADS * HEAD_DIM
WINDOW = 128
BLOCK = 128
N_BRANCHES = 2
Q_COLS = D_ATTN
KV_COLS = N_KV_HEADS * HEAD_DIM
GATE_COLS = N_BRANCHES * D_MODEL
IN_COLS = Q_COLS + 2 * KV_COLS + D_SSM + GATE_COLS
N_EXPERT_GROUPS = 4
EXPERTS_PER_GROUP = 8
N_EXPERTS = N_EXPERT_GROUPS * EXPERTS_PER_GROUP
TOP_K = 2
D_FF_EXPERT = D_MODEL // 2

kernel_name = "hybrid_s5_swa_sink_hmoe_block"


def rmsnorm(x, g):
    xf = x.astype(jnp.float32)
    inv = lax.rsqrt(jnp.mean(xf * xf, axis=-1, keepdims=True) + EPS)
    return (xf * inv * g.astype(jnp.float32)).astype(x.dtype)


def sliding_window_attention(q, k, v, sinks):
    b, l = q.shape[0], q.shape[1]
    nb = l // BLOCK
    qb = q.reshape(b, nb, BLOCK, N_KV_HEADS, Q_PER_KV, HEAD_DIM)

    def band(t):
        tb = t.reshape(b, nb, BLOCK, N_KV_HEADS, HEAD_DIM)
        prev = jnp.pad(tb[:, :-1], ((0, 0), (1, 0), (0, 0), (0, 0), (0, 0)))
        return jnp.concatenate([prev, tb], axis=2)

    kb, vb = band(k), band(v)
    scale = 1.0 / math.sqrt(HEAD_DIM)
    s = jnp.einsum('bnqkgd,bnskd->bnkgqs', qb, kb,
                   preferred_element_type=jnp.float32) * scale
    blk = jnp.arange(nb)[:, None, None] * BLOCK
    qpos = blk + jnp.arange(BLOCK)[None, :, None]
    kpos = blk - BLOCK + jnp.arange(2 * BLOCK)[None, None, :]
    rel = qpos - kpos
    mask = (rel >= 0) & (rel < WINDOW) & (kpos >= 0)
    s = jnp.where(mask[None, :, None, None], s, jnp.finfo(jnp.float32).min)
    sink = sinks.astype(jnp.float32).reshape(1, 1, N_KV_HEADS, Q_PER_KV, 1, 1)
    m = jnp.maximum(jnp.max(s, axis=-1, keepdims=True), sink)
    p = jnp.exp(s - m)
    denom = jnp.sum(p, axis=-1, keepdims=True) + jnp.exp(sink - m)
    p = (p / denom).astype(v.dtype)
    o = jnp.einsum('bnkgqs,bnskd->bnqkgd', p, vb)
    return o.reshape(b, l, D_ATTN)


def s5_ssm(u, a_re, a_im, b_re, b_im, c_re, c_im, d_skip, log_dt):
    b, l = u.shape[0], u.shape[1]
    f32 = jnp.float32
    uf = u.astype(f32).reshape(b, l, N_SSM_GROUPS, SSM_GROUP)
    lam = lax.complex(a_re.astype(f32), a_im.astype(f32))
    dt = jnp.exp(log_dt.astype(f32))[:, None]
    lam_bar = jnp.exp(lam * dt)
    b_mat = lax.complex(b_re.astype(f32), b_im.astype(f32))
    b_bar = ((lam_bar - 1.0) / lam)[:, :, None] * b_mat
    bu = jnp.einsum('blgc,gpc->blgp', uf.astype(jnp.complex64), b_bar)
    a = jnp.broadcast_to(lam_bar, bu.shape)

    def combine(left, right):
        a_l, b_l = left
        a_r, b_r = right
        return a_l * a_r, a_r * b_l + b_r

    _, states = lax.associative_scan(combine, (a, bu), axis=1)
    c_mat = lax.complex(c_re.astype(f32), c_im.astype(f32))
    y = jnp.real(jnp.einsum('blgp,gcp->blgc', states, c_mat))
    y = y + d_skip.astype(f32).reshape(N_SSM_GROUPS, SSM_GROUP) * uf
    return y.reshape(b, l, D_SSM)


def hierarchical_moe(h, w_rg, b_rg, w_re, b_re, w_gate, w_up, w_down):
    b, l, d = h.shape
    n_tok = b * l
    t = h.reshape(n_tok, d)
    group_logits = (t @ w_rg).astype(jnp.float32) + b_rg.astype(jnp.float32)
    group_probs = jax.nn.softmax(group_logits, axis=-1)
    group_p, group_idx = lax.top_k(group_probs, 1)
    expert_logits = ((t @ w_re).astype(jnp.float32) + b_re.astype(jnp.float32)
                     ).reshape(n_tok, N_EXPERT_GROUPS, EXPERTS_PER_GROUP)
    sel_logits = jnp.take_along_axis(expert_logits, group_idx[:, :, None], axis=1)[:, 0]
    within = jax.nn.softmax(sel_logits, axis=-1)
    w_top, e_local = lax.top_k(within, TOP_K)
    w_top = w_top / jnp.sum(w_top, axis=-1, keepdims=True) * group_p
    expert_ids = group_idx * EXPERTS_PER_GROUP + e_local
    flat_e = expert_ids.reshape(-1)
    order = jnp.argsort(flat_e)
    tok = order // TOP_K
    xs = t[tok]
    sizes = jnp.bincount(flat_e, length=N_EXPERTS).astype(jnp.int32)
    hg = lax.ragged_dot(xs, w_gate, sizes)
    hu = lax.ragged_dot(xs, w_up, sizes)
    ys = lax.ragged_dot(jax.nn.silu(hg) * hu, w_down, sizes)
    ws = w_top.reshape(-1)[order].astype(ys.dtype)
    out = jax.ops.segment_sum(ys * ws[:, None], tok, num_segments=n_tok)
    return out.reshape(b, l, d).astype(h.dtype)


def setup_inputs(seed: int = 0) -> dict:
    key = jax.random.key(seed)
    ks = jax.random.split(key, 32)
    f32 = jnp.float32
    L, D = DEPTH, D_MODEL
    G, P, C = N_SSM_GROUPS, SSM_STATE, SSM_GROUP

    def nrm(k, shape, scale):
        return jax.random.normal(k, shape, f32) * scale

    n_idx = jnp.arange(P, dtype=f32)
    a_re = -0.5 + 0.01 * jax.random.normal(ks[4], (L, G, P), f32)
    a_im = math.pi * n_idx[None, None, :] + 0.01 * jax.random.normal(ks[5], (L, G, P), f32)
    log_dt = jax.random.uniform(ks[12], (L, G), f32, math.log(DT_MIN), math.log(DT_MAX))
    return {
        "x": nrm(ks[0], (BATCH, SEQ, D), 1.0),
        "norm_mix": 1.0 + nrm(ks[1], (L, D), 0.02),
        "w_in": nrm(ks[2], (L, D, IN_COLS), D ** -0.5),
        "b_gate": nrm(ks[3], (L, GATE_COLS), 0.01),
        "attn_sinks": nrm(ks[13], (L, N_HEADS), 1.0),
        "ssm_a_re": a_re,
        "ssm_a_im": a_im,
        "ssm_b_re": nrm(ks[6], (L, G, P, C), (2 * C) ** -0.5),
        "ssm_b_im": nrm(ks[7], (L, G, P, C), (2 * C) ** -0.5),
        "ssm_c_re": nrm(ks[8], (L, G, C, P), (2 * P) ** -0.5),
        "ssm_c_im": nrm(ks[9], (L, G, C, P), (2 * P) ** -0.5),
        "ssm_d": nrm(ks[10], (L, D_SSM), 1.0),
        "ssm_log_dt": log_dt,
        "w_glu": nrm(ks[11], (L, D_SSM, D_SSM), D_SSM ** -0.5),
        "b_glu": nrm(ks[14], (L, D_SSM), 0.01),
        "w_attn_branch": nrm(ks[15], (L, D_ATTN, D), D_ATTN ** -0.5),
        "w_ssm_branch": nrm(ks[16], (L, D_SSM, D), D_SSM ** -0.5),
        "w_out": nrm(ks[17], (L, D, D), D ** -0.5),
        "norm_moe": 1.0 + nrm(ks[18], (L, D), 0.02),
        "w_router_group": nrm(ks[19], (L, D, N_EXPERT_GROUPS), D ** -0.5),
        "b_router_group": nrm(ks[20], (L, N_EXPERT_GROUPS), 0.01),
        "w_router_expert": nrm(ks[21], (L, D, N_EXPERTS), D ** -0.5),
        "b_router_expert": nrm(ks[22], (L, N_EXPERTS), 0.01),
        "w_expert_gate": nrm(ks[23], (L, N_EXPERTS, D, D_FF_EXPERT), D ** -0.5),
        "w_expert_up": nrm(ks[24], (L, N_EXPERTS, D, D_FF_EXPERT), D ** -0.5),
        "w_expert_down": nrm(ks[25], (L, N_EXPERTS, D_FF_EXPERT, D), D_FF_EXPERT ** -0.5),
        "norm_final": 1.0 + nrm(ks[26], (D,), 0.02),
    }


def reference(x, norm_mix, w_in, b_gate, attn_sinks, ssm_a_re, ssm_a_im, ssm_b_re, ssm_b_im,
              ssm_c_re, ssm_c_im, ssm_d, ssm_log_dt, w_glu, b_glu, w_attn_branch, w_ssm_branch,
              w_out, norm_moe, w_router_group, b_router_group, w_router_expert, b_router_expert,
              w_expert_gate, w_expert_up, w_expert_down, norm_final):
    b, l, _ = x.shape
    splits = [Q_COLS, Q_COLS + KV_COLS, Q_COLS + 2 * KV_COLS, Q_COLS + 2 * KV_COLS + D_SSM]
    for i in range(DEPTH):
        h = rmsnorm(x, norm_mix[i])
        proj = h @ w_in[i]
        q, k, v, u, gate_logits = jnp.split(proj, splits, axis=-1)
        q = q.reshape(b, l, N_HEADS, HEAD_DIM)
        k = k.reshape(b, l, N_KV_HEADS, HEAD_DIM)
        v = v.reshape(b, l, N_KV_HEADS, HEAD_DIM)
        attn = sliding_window_attention(q, k, v, attn_sinks[i])
        y = s5_ssm(u, ssm_a_re[i], ssm_a_im[i], ssm_b_re[i], ssm_b_im[i],
                   ssm_c_re[i], ssm_c_im[i], ssm_d[i], ssm_log_dt[i]).astype(x.dtype)
        z = jax.nn.gelu(y)
        z = z * jax.nn.sigmoid(z @ w_glu[i] + b_glu[i])
        gates = jax.nn.sigmoid(gate_logits.astype(jnp.float32) + b_gate[i].astype(jnp.float32))
        g_attn, g_ssm = jnp.split(gates.astype(x.dtype), 2, axis=-1)
        merged = g_attn * (attn @ w_attn_branch[i]) + g_ssm * (z @ w_ssm_branch[i])
        x = x + merged @ w_out[i]
        h2 = rmsnorm(x, norm_moe[i])
        x = x + hierarchical_moe(h2, w_router_group[i], b_router_group[i], w_router_expert[i],
                                 b_router_expert[i], w_expert_gate[i], w_expert_up[i],
                                 w_expert_down[i])
    return rmsnorm(x, norm_final)
```

```python
import math, contextlib, os
DBG_TG = int(os.environ.get("DBG_TG", "0"))
import numpy as np
import concourse.bass as bass
import concourse.mybir as mybir
from concourse.bass_utils import run_bass_kernel_spmd

F32 = mybir.dt.float32
F32R = mybir.dt.float32r
AF = mybir.ActivationFunctionType
ALU = mybir.AluOpType
AX = mybir.AxisListType

PE, ACT, DVE, POOL, SP = "pe", "act", "dve", "pool", "sp"
ENGS = [PE, ACT, DVE, POOL, SP]
NDSEM = 14
EPS = 1e-6
NT = 64
NOWN = 2048
NALL = 8192
WCOLS = 512 + 512 + 128 + 1024 + 2048
CQ, CK, CV, CU, CG = 0, 512, 1024, 1152, 2176
NEG = -30000.0
CAP = 256
NROW = 32 * CAP


class _Rec:
    def __init__(self):
        self.calls = []

    def __getattr__(self, name):
        def f(*a, **k):
            self.calls.append((name, a, k))
            return self
        return f


def _record(fn):
    r = _Rec()
    fn(r)
    assert len(r.calls) == 1, r.calls
    return r.calls[0]


class _Stop(Exception):
    pass


class Prog:
    def __init__(self, nc):
        self.nc = nc
        self.q = {e: [] for e in ENGS}
        self.cnt = {e: 0 for e in ENGS}
        self.res = {}
        self.known = {e: {} for e in ENGS}
        self.dcnt = [0] * NDSEM
        self.dnext = 0

    def _deps(self, eng, reads, writes):
        deps = {}

        def add(d):
            if d is None:
                return
            k, v = d
            if deps.get(k, 0) < v:
                deps[k] = v

        for r in reads:
            st = self.res.get(r)
            if st:
                add(st["w"])
        for w in writes:
            st = self.res.get(w)
            if st:
                add(st["w"])
                for d in st["r"]:
                    add(d)
        out = []
        for k, v in deps.items():
            if eng == PE and k == PE:
                continue
            if self.known[eng].get(k, 0) >= v:
                continue
            self.known[eng][k] = v
            out.append((k, v))
        return out

    def _mark(self, reads, writes, tok):
        for r in reads:
            st = self.res.setdefault(r, {"w": None, "r": []})
            st["r"].append(tok)
        for w in writes:
            self.res[w] = {"w": tok, "r": []}

    def op(self, eng, fn, reads=(), writes=()):
        deps = self._deps(eng, reads, writes)
        self.cnt[eng] += 1
        tok = (eng, self.cnt[eng])
        self.q[eng].append(("op", _record(fn), deps))
        self._mark(reads, writes, tok)
        return tok

    def dma(self, eng, fn, reads=(), writes=()):
        deps = self._deps(eng, reads, writes)
        s = self.dnext
        self.dnext = (self.dnext + 1) % NDSEM
        prev = self.dcnt[s]
        k = ("d", s)
        if prev and self.known[eng].get(k, 0) < prev:
            self.known[eng][k] = prev
            deps.append((k, prev))
        self.dcnt[s] += 16
        tok = (k, self.dcnt[s])
        self.q[eng].append(("dma", _record(fn), deps, s))
        self._mark(reads, writes, tok)
        return tok

    def barrier(self):
        for e in ENGS:
            deps = []
            for e2 in [PE, ACT, DVE, POOL]:
                v = self.cnt[e2]
                if v and self.known[e].get(e2, 0) < v and e2 != e:
                    self.known[e][e2] = v
                    deps.append((e2, v))
            for s in range(NDSEM):
                v = self.dcnt[s]
                k = ("d", s)
                if v and self.known[e].get(k, 0) < v:
                    self.known[e][k] = v
                    deps.append((k, v))
            self.q[e].append(("wait", None, deps))

    def emit(self):
        nc = self.nc
        if getattr(self, "flag", None) is not None:
            dflag, src = self.flag
            self.barrier()
            self.dma(SP, lambda e: e.dma_start(out=dflag, in_=src), writes=["dflag"])
            self.barrier()
        with contextlib.ExitStack() as st:
            sem = {}
            for e in [PE, ACT, DVE, POOL]:
                sem[e] = st.enter_context(nc.semaphore("s_" + e))
            for s in range(NDSEM):
                sem[("d", s)] = st.enter_context(nc.semaphore("s_d%d" % s))
            block = st.enter_context(nc.Block())

            def run(ename, eobj):
                bc_reg = None
                for item in self.q[ename]:
                    kind, fn, deps = item[0], item[1], item[2]
                    if kind == "dma" and fn[0] == "indirect_dma_start" and fn[2].get("bounds_check") is not None:
                        if bc_reg is None:
                            bc_reg = eobj.to_reg(fn[2]["bounds_check"])
                        kw = dict(fn[2]); kw["bounds_check"] = bc_reg
                        fn = (fn[0], fn[1], kw)
                    for k, v in deps:
                        eobj.wait_ge(sem[k], v)
                    if kind == "op":
                        getattr(eobj, fn[0])(*fn[1], **fn[2]).then_inc(sem[ename], 1)
                    elif kind == "dma":
                        getattr(eobj, fn[0])(*fn[1], **fn[2]).then_inc(sem[("d", item[3])], 16)

            @block.tensor
            def _(e):
                run(PE, e)

            @block.scalar
            def _(e):
                run(ACT, e)

            @block.vector
            def _(e):
                run(DVE, e)

            @block.gpsimd
            def _(e):
                run(POOL, e)

            @block.sync
            def _(e):
                run(SP, e)


def build_nc(stop=99, debug=False):
    nc = bass.Bass("TRN2", target_bir_lowering=False)

    def din(name, shape):
        return nc.dram_tensor(name, shape, F32, kind="ExternalInput").ap()

    def dscr(name, shape, ph=0):
        return nc.dram_tensor(name, shape, F32, kind="ExternalOutput" if (debug and stop == ph) else "Internal").ap()

    x_all = din("x_all", [NALL, 1024])
    w_in = din("w_in", [1024, WCOLS])
    gmix = din("gmix", [128, 8]); gmoe = din("gmoe", [128, 8]); gfin = din("gfin", [128, 1024])
    bgate = din("bgate", [128, 16]); sinks = din("sinks", [128, 8])
    m3_d = din("m3", [128, 1]); ident_d = din("ident", [128, 128]); jswap_d = din("jswap", [128, 128]); sgn_d = din("sgn", [128, 1])
    mcur_d = din("mcur", [128, 512]); mprev_d = din("mprev", [128, 512]); mprev0_d = din("mprev0", [128, 512])
    are1 = din("are1", [128, 32]); aim1 = din("aim1", [128, 32]); ldt1 = din("ldt1", [128, 32])
    are2 = din("are2", [128, 512]); aim2 = din("aim2", [128, 512]); ldt2 = din("ldt2", [128, 512])
    bre2 = din("bre2", [128, 512]); bim2 = din("bim2", [128, 512])
    cst_d = din("cst", [128, 512]); dpad_d = din("dpad", [128, 8])
    wglu = din("wglu", [1024, 1024]); bglu = din("bglu", [128, 8])
    wab = din("wab", [512, 1024]); wsb = din("wsb", [1024, 1024]); wout = din("wout", [1024, 1024])
    wr = din("wr", [128, 8, 36]); br = din("br", [128, 36])
    ustr_d = din("ustr", [128, 128]); ones_d = din("ones", [128, 128]); ecap_d = din("ecap", [128, 32])
    if stop > 3:
        weg = din("weg", [32, 1024, 512]); weu = din("weu", [32, 1024, 512]); wed = din("wed", [32, 512, 1024])
        y_out = nc.dram_tensor("y_out", [NOWN, 1024], F32, kind="ExternalOutput").ap()

    HT = dscr("HT", [8, 128, 2176], 1); UT = dscr("UT", [8, 128, NALL], 1); ZT = dscr("ZT", [8, 128, NOWN], 2)
    X1 = dscr("X1", [NOWN, 1024], 3.5)
    Xscr = dscr("Xscr", [NROW, 1024], 3.5); Yscr = dscr("Yscr", [NROW, 1024], 3.5)

    P = Prog(nc)
    pcnt = [0]
    dflag = nc.dram_tensor("dflag", [128, 128], F32, kind="ExternalOutput").ap() if debug else None

    with contextlib.ExitStack() as gst:
        def sb(st, name, shape, dt=F32):
            return st.enter_context(nc.sbuf_tensor("sb_" + name, shape, dt))

        banks = [gst.enter_context(nc.psum_tensor("bank%d" % i, [128, 512], F32)) for i in range(8)]

        def bank(i):
            return banks[i], ("ps", i)

        ident = sb(gst, "ident", [128, 128]); jswap = sb(gst, "jswap", [128, 128]); sgn = sb(gst, "sgn", [128, 1])
        identr = sb(gst, "identr", [128, 128], F32R); epsT = sb(gst, "epsT", [128, 1])
        P.op(DVE, lambda e: e.memset(epsT[:], EPS), writes=["epsT"])
        if debug:
            P.flag = (dflag, ident[:])
        P.dma(SP, lambda e: e.dma_start(out=ident[:], in_=ident_d), writes=["ident"])
        P.dma(SP, lambda e: e.dma_start(out=jswap[:], in_=jswap_d), writes=["jswap"])
        P.dma(SP, lambda e: e.dma_start(out=sgn[:], in_=sgn_d), writes=["sgn"])
        P.op(DVE, lambda e: e.tensor_copy(out=identr[:], in_=ident[:]), reads=["ident"], writes=["identr"])

        def norm_T(st_bufs, src_ap_fn, i, gfull, gkey, dst, dstkey, col0, tag):
            xt, xn, nxn, ss, rs = st_bufs
            s = i % 2
            sn = i % nxn
            xk = (tag + "xt", s); nk = (tag + "xn", sn)
            if src_ap_fn is not None:
                P.dma(SP, lambda e: e.dma_start(out=xt[:, s, :], in_=src_ap_fn(i)), writes=[xk])
            P.op(ACT, lambda e: e.activation(out=xn[:, sn, :], in_=xt[:, s, :], func=AF.Square, accum_out=ss[:, i:i + 1]),
                 reads=[xk], writes=[nk, (tag + "ss", i)])
            P.op(ACT, lambda e: e.activation(out=rs[:, i:i + 1], in_=ss[:, i:i + 1], func=AF.Sqrt, bias=epsT[:, 0:1], scale=1.0 / 1024),
                 reads=[(tag + "ss", i), "epsT"], writes=[(tag + "rs", i)])
            P.op(DVE, lambda e: e.reciprocal(out=rs[:, i:i + 1], in_=rs[:, i:i + 1]), reads=[(tag + "rs", i)], writes=[(tag + "rs", i)])
            P.op(DVE, lambda e: e.tensor_scalar(out=xn[:, sn, :], in0=xt[:, s, :], scalar1=rs[:, i:i + 1], scalar2=None, op0=ALU.mult),
                 reads=[xk, (tag + "rs", i), nk], writes=[nk])
            for hh in range(2):
                bi = pcnt[0] % 2; pcnt[0] += 1
                bk, bkey = bank(bi)
                for j in range(4):
                    k = hh * 4 + j
                    P.op(PE, lambda e, k=k, j=j, bk=bk: e.transpose(out=bk[:, j * 128:(j + 1) * 128], in_=xn[:, sn, k * 128:(k + 1) * 128], identity=ident[:]),
                         reads=[nk, "ident"], writes=[bkey])
                P.op(DVE, lambda e, hh=hh, bk=bk: e.tensor_tensor(out=dst[:, hh * 4:hh * 4 + 4, col0:col0 + 128],
                                                                 in0=bk[:].rearrange("p (k t) -> p k t", k=4),
                                                                 in1=gfull[:, hh * 4:hh * 4 + 4, :], op=ALU.mult),
                     reads=[bkey, gkey], writes=[dstkey])

        def make_gfull(st, name, g_d):
            g2 = sb(st, name + "2", [128, 8]); gf = sb(st, name + "f", [128, 8, 128])
            P.dma(SP, lambda e: e.dma_start(out=g2[:], in_=g_d), writes=[name + "2"])
            P.op(POOL, lambda e: e.memset(gf[:], 1.0), writes=[name])
            for k in range(8):
                P.op(POOL, lambda e, k=k: e.tensor_scalar(out=gf[:, k, :], in0=gf[:, k, :], scalar1=g2[:, k:k + 1], scalar2=None, op0=ALU.mult),
                     reads=[name + "2", name], writes=[name])
            return gf

        def wload(dst_ap, src_ap, key, reads=()):
            P.dma(POOL, lambda e: e.dma_start(out=dst_ap, in_=src_ap, max_dma_last_dim=8192), reads=list(reads), writes=[key])

        sst = contextlib.ExitStack()
        cs = sb(sst, "cs", [128, 13, 32]); dsg = sb(sst, "dsg", [128, 13, 32])
        Bst = sb(sst, "Bst", [128, 8, 128], F32R)
        Bst3 = sb(sst, "Bst3", [128, 8, 128], F32R); m3 = sb(sst, "m3", [128, 1])
        P.dma(SP, lambda e: e.dma_start(out=m3[:], in_=m3_d), writes=["m3"])
        Cpad = sb(sst, "Cpad", [128, 32, 128], F32R)
        Dpad = sb(sst, "Dpad", [128, 8])
        P.dma(SP, lambda e: e.dma_start(out=Dpad[:], in_=dpad_d), writes=["Dpad"])

        with contextlib.ExitStack() as st:
            def lam_bar(n, are_d, aim_d, ldt_d, tag):
                t = {nm: sb(st, tag + nm, [128, n]) for nm in ["are", "aim", "dt", "mag", "ang", "r", "c", "d", "x"]}
                t["qi"] = sb(st, tag + "qi", [128, n], mybir.dt.int32)
                P.dma(SP, lambda e: e.dma_start(out=t["are"][:], in_=are_d), writes=[tag + "are"])
                P.dma(SP, lambda e: e.dma_start(out=t["aim"][:], in_=aim_d), writes=[tag + "aim"])
                P.dma(SP, lambda e: e.dma_start(out=t["dt"][:], in_=ldt_d), writes=[tag + "dt"])
                K = lambda *a: [tag + x for x in a]
                P.op(ACT, lambda e: e.activation(out=t["dt"][:], in_=t["dt"][:], func=AF.Exp), reads=K("dt"), writes=K("dt"))
                P.op(DVE, lambda e: e.tensor_tensor(out=t["mag"][:], in0=t["are"][:], in1=t["dt"][:], op=ALU.mult), reads=K("are", "dt"), writes=K("mag"))
                P.op(ACT, lambda e: e.activation(out=t["mag"][:], in_=t["mag"][:], func=AF.Exp), reads=K("mag"), writes=K("mag"))
                P.op(DVE, lambda e: e.tensor_tensor(out=t["ang"][:], in0=t["aim"][:], in1=t["dt"][:], op=ALU.mult), reads=K("aim", "dt"), writes=K("ang"))
                for nm, off in (("d", 0.0), ("c", 0.5 * math.pi)):
                    P.op(DVE, lambda e, off=off: e.tensor_scalar(out=t["x"][:], in0=t["ang"][:], scalar1=off, scalar2=None, op0=ALU.add), reads=K("ang"), writes=K("x"))
                    P.op(DVE, lambda e: e.tensor_scalar(out=t["r"][:], in0=t["x"][:], scalar1=1.0 / (2 * math.pi), scalar2=None, op0=ALU.mult), reads=K("x"), writes=K("r"))
                    P.op(DVE, lambda e: e.tensor_copy(out=t["qi"][:], in_=t["r"][:]), reads=K("r"), writes=K("qi"))
                    P.op(DVE, lambda e: e.tensor_copy(out=t["r"][:], in_=t["qi"][:]), reads=K("qi"), writes=K("r"))
                    P.op(DVE, lambda e: e.scalar_tensor_tensor(out=t["r"][:], in0=t["r"][:], scalar=-2 * math.pi, in1=t["x"][:], op0=ALU.mult, op1=ALU.add), reads=K("r", "x"), writes=K("r"))
                    P.op(DVE, lambda e: e.tensor_scalar(out=t["x"][:], in0=t["r"][:], scalar1=math.pi, scalar2=2 * math.pi, op0=ALU.is_gt, op1=ALU.mult), reads=K("r"), writes=K("x"))
                    P.op(DVE, lambda e: e.tensor_tensor(out=t["r"][:], in0=t["r"][:], in1=t["x"][:], op=ALU.subtract), reads=K("r", "x"), writes=K("r"))
                    P.op(DVE, lambda e: e.tensor_scalar(out=t["r"][:], in0=t["r"][:], scalar1=math.pi, scalar2=-math.pi, op0=ALU.min, op1=ALU.max), reads=K("r"), writes=K("r"))
                    P.op(ACT, lambda e, nm=nm: e.activation(out=t[nm][:], in_=t["r"][:], func=AF.Sin), reads=K("r"), writes=K(nm))
                    P.op(DVE, lambda e, nm=nm: e.tensor_tensor(out=t[nm][:], in0=t[nm][:], in1=t["mag"][:], op=ALU.mult), reads=K(nm, "mag"), writes=K(nm))
                return t

            t1 = lam_bar(32, are1, aim1, ldt1, "l1")
            tmpa = sb(st, "tmpa", [128, 32]); tmpb = sb(st, "tmpb", [128, 32]); dd = sb(st, "dd", [128, 13, 32])
            P.op(DVE, lambda e: e.tensor_copy(out=cs[:, 0, :], in_=t1["c"][:]), reads=["l1c"], writes=["cs"])
            P.op(DVE, lambda e: e.tensor_copy(out=dd[:, 0, :], in_=t1["d"][:]), reads=["l1d"], writes=["dd"])
            for k in range(1, 13):
                P.op(DVE, lambda e, k=k: e.tensor_tensor(out=tmpa[:], in0=cs[:, k - 1, :], in1=cs[:, k - 1, :], op=ALU.mult), reads=["cs"], writes=["tmpa"])
                P.op(DVE, lambda e, k=k: e.tensor_tensor(out=tmpb[:], in0=dd[:, k - 1, :], in1=dd[:, k - 1, :], op=ALU.mult), reads=["dd"], writes=["tmpb"])
                P.op(DVE, lambda e, k=k: e.scalar_tensor_tensor(out=dd[:, k, :], in0=cs[:, k - 1, :], scalar=2.0, in1=dd[:, k - 1, :], op0=ALU.mult, op1=ALU.mult),
                     reads=["cs", "dd"], writes=["dd"])
                P.op(DVE, lambda e, k=k: e.tensor_tensor(out=cs[:, k, :], in0=tmpa[:], in1=tmpb[:], op=ALU.subtract), reads=["tmpa", "tmpb", "cs"], writes=["cs"])
            P.op(DVE, lambda e: e.tensor_scalar(out=dsg[:], in0=dd[:], scalar1=sgn[:, 0:1], scalar2=None, op0=ALU.mult), reads=["dd", "sgn"], writes=["dsg"])

            t2 = lam_bar(512, are2, aim2, ldt2, "l2")
            br_t = sb(st, "br_t", [128, 512]); bi_t = sb(st, "bi_t", [128, 512])
            P.dma(SP, lambda e: e.dma_start(out=br_t[:], in_=bre2), writes=["br_t"])
            P.dma(SP, lambda e: e.dma_start(out=bi_t[:], in_=bim2), writes=["bi_t"])
            den = sb(st, "den", [128, 512]); u1 = sb(st, "u1", [128, 512]); u2 = sb(st, "u2", [128, 512])
            kr = sb(st, "kr", [128, 512]); ki = sb(st, "ki", [128, 512])
            TT = lambda o, a, b, op, rd, wr_: P.op(DVE, lambda e: e.tensor_tensor(out=o, in0=a, in1=b, op=op), reads=rd, writes=wr_)
            TT(den[:], t2["are"][:], t2["are"][:], ALU.mult, ["l2are"], ["den"])
            TT(u1[:], t2["aim"][:], t2["aim"][:], ALU.mult, ["l2aim"], ["u1"])
            TT(den[:], den[:], u1[:], ALU.add, ["den", "u1"], ["den"])
            P.op(DVE, lambda e: e.reciprocal(out=den[:], in_=den[:]), reads=["den"], writes=["den"])
            P.op(DVE, lambda e: e.tensor_scalar(out=t2["c"][:], in0=t2["c"][:], scalar1=-1.0, scalar2=None, op0=ALU.add), reads=["l2c"], writes=["l2c"])
            TT(u1[:], t2["c"][:], t2["are"][:], ALU.mult, ["l2c", "l2are"], ["u1"])
            TT(u2[:], t2["d"][:], t2["aim"][:], ALU.mult, ["l2d", "l2aim"], ["u2"])
            TT(kr[:], u1[:], u2[:], ALU.add, ["u1", "u2"], ["kr"])
            TT(kr[:], kr[:], den[:], ALU.mult, ["kr", "den"], ["kr"])
            TT(u1[:], t2["d"][:], t2["are"][:], ALU.mult, ["l2d", "l2are"], ["u1"])
            TT(u2[:], t2["c"][:], t2["aim"][:], ALU.mult, ["l2c", "l2aim"], ["u2"])
            TT(ki[:], u1[:], u2[:], ALU.subtract, ["u1", "u2"], ["ki"])
            TT(ki[:], ki[:], den[:], ALU.mult, ["ki", "den"], ["ki"])
            v3 = lambda a: a[:].rearrange("p (q n) -> p q n", q=8)
            TT(u1[:], kr[:], br_t[:], ALU.mult, ["kr", "br_t"], ["u1"])
            TT(u2[:], ki[:], bi_t[:], ALU.mult, ["ki", "bi_t"], ["u2"])
            TT(Bst[:, :, 0:64], v3(u1), v3(u2), ALU.subtract, ["u1", "u2"], ["Bst"])
            TT(u1[:], kr[:], bi_t[:], ALU.mult, ["kr", "bi_t"], ["u1"])
            TT(u2[:], ki[:], br_t[:], ALU.mult, ["ki", "br_t"], ["u2"])
            TT(Bst[:, :, 64:128], v3(u1), v3(u2), ALU.add, ["u1", "u2", "Bst"], ["Bst"])
            P.op(DVE, lambda e: e.tensor_scalar(out=Bst3[:], in0=Bst[:].bitcast(F32), scalar1=m3[:, 0:1], scalar2=None, op0=ALU.mult), reads=["Bst", "m3"], writes=["Bst3"])
            cst = sb(st, "cst", [128, 32, 16])
            P.dma(SP, lambda e: e.dma_start(out=cst[:].rearrange("p g c -> p (g c)"), in_=cst_d), writes=["cst"])
            for g_ in range(32):
                P.op(POOL, lambda e, g_=g_: e.tensor_scalar(out=Cpad[:, g_, :], in0=ident[:], scalar1=0.0, scalar2=None, op0=ALU.mult), reads=["ident", "Cpad"], writes=["Cpad"])
            for gi in range(4):
                P.op(DVE, lambda e, gi=gi: e.tensor_copy(out=Cpad[0:64, gi::4, 32 * gi:32 * gi + 16], in_=cst[0:64, gi::4, :]), reads=["cst", "Cpad"], writes=["Cpad"])
                P.op(DVE, lambda e, gi=gi: e.tensor_scalar(out=Cpad[64:128, gi::4, 32 * gi:32 * gi + 16], in0=cst[64:128, gi::4, :], scalar1=-1.0, scalar2=None, op0=ALU.mult),
                     reads=["cst", "Cpad"], writes=["Cpad"])

            zt = sb(st, "zt", [128, 2048])
            P.op(DVE, lambda e: e.memset(zt[:], 0.0), writes=["zt"])
            xz = Xscr.rearrange("(p r) c -> p r c", p=128)
            for r_ in range(0, 64, 2):
                P.dma(SP, lambda e, r_=r_: e.dma_start(out=xz[:, r_:r_ + 2, :], in_=zt[:].rearrange("p (r c) -> p r c", r=2)), reads=["zt"], writes=[("Xz", r_)])
            gmixf = make_gfull(st, "gmix", gmix)
            xt = sb(st, "a_xt", [128, 2, 1024]); xn = sb(st, "a_xn", [128, 2, 1024]); junk = 2
            ss = sb(st, "a_ss", [128, NT]); rs = sb(st, "a_rs", [128, NT])
            bufs = (xt, xn, junk, ss, rs)
            wU = sb(st, "wU", [128, 8, 1024], F32R)
            wload(wU[:], w_in.rearrange("(k p) c -> p k c", p=128)[:, :, CU:CU + 1024], "wU")
            hTg = sb(st, "hTg", [128, 2, 8, 512], F32R); UTg = sb(st, "UTg", [128, 2, 8, 512])
            for grp in range(16):
                gs = grp % 2
                hk = ("hTg", gs)
                for j in range(4):
                    i = grp * 4 + j
                    norm_T(bufs, lambda i: x_all[i * 128:(i + 1) * 128, :], i, gmixf, "gmix", hTg[:, gs], hk, j * 128, "a_")
                for ch in range(8):
                    bi = 2 + (ch % 4)
                    bk, bkey = bank(bi)
                    for k in range(8):
                        P.op(PE, lambda e, k=k, ch=ch, bk=bk: e.matmul(bk[:], wU[:, k, ch * 128:(ch + 1) * 128], hTg[:, gs, k, :], start=(k == 0), stop=(k == 7)),
                             reads=["wU", hk], writes=[bkey])
                    P.op(ACT, lambda e, ch=ch, bk=bk: e.copy(out=UTg[:, gs, ch, :], in_=bk[:]), reads=[bkey], writes=[("UTg", gs)])
                for gi_ in range(4):
                    P.dma(ACT, lambda e, grp=grp, gi_=gi_: e.dma_start(out=UT[:, 32 * gi_:32 * gi_ + 16, grp * 512:(grp + 1) * 512].rearrange("k p t -> p k t"), in_=UTg[32 * gi_:32 * gi_ + 16, gs]),
                          reads=[("UTg", gs)], writes=[("UT", grp, gi_)])
                if grp == 11:
                    P.dma(ACT, lambda e: e.dma_start(out=HT[:, :, 0:128].rearrange("k p t -> p k t"), in_=hTg[:, gs, :, 384:512].bitcast(F32)),
                          reads=[hk], writes=[("HT", 0)])
                if grp >= 12:
                    c0 = 128 + (grp - 12) * 512
                    P.dma(ACT, lambda e, c0=c0: e.dma_start(out=HT[:, :, c0:c0 + 512].rearrange("k p t -> p k t"), in_=hTg[:, gs].bitcast(F32)),
                          reads=[hk], writes=[("HT", grp - 11)])
        P.barrier()
        if stop <= 1:
            sst.close(); P.emit(); globals()["LASTP"] = P; return nc

        with contextlib.ExitStack() as st:
            UTc = sb(st, "UTc", [128, NALL], F32R)
            for c_ in range(0, NALL, 128):
                P.op(POOL if (c_ // 128) % 2 else DVE, lambda e, c_=c_: e.tensor_scalar(out=UTc[:, c_:c_ + 128], in0=ident[:], scalar1=0.0, scalar2=None, op0=ALU.mult), reads=["ident"], writes=["UTc"])
            Xs = [sb(st, "X%d" % i, [128, NALL], F32R) for i in range(4)]
            ATl = sb(st, "ATl", [128, 2, 4, 128], F32R); lvl = [0]
            tmpJ4 = sb(st, "tmpJ4", [128, 4, 128])
            yv = sb(st, "yv", [128, 2, 512]); ta = sb(st, "ta", [128, 2, 512]); zs = ta
            ycnt = 0
            for q in range(8):
                for gi_ in range(4):
                    P.dma(POOL, lambda e, gi_=gi_: e.dma_start(out=UTc[32 * gi_:32 * gi_ + 16, :], in_=UT[q][32 * gi_:32 * gi_ + 16, :], max_dma_last_dim=8192),
                          reads=[("UT", g_, gi_) for g_ in range(16)] + ["UTc"], writes=[("UTc", gi_)])
                for gi in range(4):
                    X = Xs[gi]
                    for b in range(16):
                        bi = b % 4
                        bk, bkey = bank(bi)
                        P.op(PE, lambda e, b=b, bk=bk, X=X, gi=gi: e.matmul(bk[:], (Bst[32 * gi:32 * gi + 32, q, :] if gi < 3 else Bst3[64:128, q, :]), (UTc[32 * gi:32 * gi + 32, b * 512:(b + 1) * 512] if gi < 3 else UTc[64:128, b * 512:(b + 1) * 512]), start=True, stop=True),
                             reads=["Bst", "Bst3", "UTc"] + [("UTc", g_) for g_ in range(4)], writes=[bkey])
                        if b % 2 == 0:
                            P.op(ACT, lambda e, b=b, bk=bk, X=X: e.copy(out=X[:, b * 512:(b + 1) * 512], in_=bk[:]), reads=[bkey], writes=[("X", gi, b)])
                        else:
                            P.op(DVE, lambda e, b=b, bk=bk, X=X: e.tensor_copy(out=X[:, b * 512:(b + 1) * 512], in_=bk[:]), reads=[bkey], writes=[("X", gi, b)])
                def flush(pend, stride):
                    for gi_, X_, bk_, bkey_, t0, na in pend:
                        P.op(DVE, lambda e, bk_=bk_, X_=X_, t0=t0, na=na, stride=stride: e.tensor_tensor(out=X_[:, t0:t0 + (na - 1) * stride + 1:stride], in0=bk_[:, 0:na], in1=X_[:, t0:t0 + (na - 1) * stride + 1:stride].bitcast(F32), op=ALU.add),
                             reads=[bkey_, ("Xg", gi_)], writes=[("Xg", gi_)])
                    del pend[:]

                levels = [(1 << k, True) for k in range(13)] + [(1 << k, False) for k in range(11, -1, -1)]
                for li, (d, up) in enumerate(levels):
                    k = d.bit_length() - 1
                    slot = lvl[0] % 2; lvl[0] += 1
                    for gi in range(4):
                        g = 4 * q + gi
                        P.op(ACT, lambda e, k=k, g=g, gi=gi: e.activation(out=tmpJ4[:, gi, :], in_=jswap[:], func=AF.Copy, scale=dsg[:, k, g:g + 1]),
                             reads=["jswap", "dsg"], writes=[("tmpJ4", gi)])
                        P.op(DVE, lambda e, k=k, g=g, slot=slot, gi=gi: e.scalar_tensor_tensor(out=ATl[:, slot, gi, :], in0=ident[:], scalar=cs[:, k, g:g + 1], in1=tmpJ4[:, gi, :], op0=ALU.mult, op1=ALU.add),
                             reads=["ident", "cs", ("tmpJ4", gi)], writes=[("ATl", slot, gi)])
                    if up:
                        src0, dst0, stride, cnt = d - 1, 2 * d - 1, 2 * d, NALL // (2 * d)
                    elif d == 2048:
                        src0, dst0, stride, cnt = 4095, 6143, 4096, 1
                    else:
                        src0, dst0, stride, cnt = 6143, 6143 + d, 2 * d, 1024 // d
                    if cnt == 1:
                        stride = NALL - 1 - src0
                    cnt_mm = cnt + (cnt % 2)
                    pend = []
                    for gi in range(4):
                        X = Xs[gi]
                        for p0 in range(0, cnt_mm, 512):
                            n = min(512, cnt_mm - p0)
                            na = min(n, cnt - p0)
                            bi = 4 + (pcnt[0] % 4); pcnt[0] += 1
                            bk, bkey = bank(bi)
                            a0 = src0 + p0 * stride
                            rd = [("Xg", gi), ("ATl", slot, gi)] + ([("X", gi, b_) for b_ in range(16)] if li == 0 else [])
                            P.op(PE, lambda e, bk=bk, X=X, a0=a0, n=n, stride=stride, slot=slot, gi=gi: e.matmul(bk[:, 0:n], ATl[:, slot, gi, :], X[:, a0:a0 + (n - 1) * stride + 1:stride], start=True, stop=True),
                                 reads=rd, writes=[bkey])
                            pend.append((gi, X, bk, bkey, dst0 + p0 * stride, na))
                            if len(pend) == 4:
                                flush(pend, stride)
                    flush(pend, stride)
                for tb in range(4):
                    c0 = 6144 + tb * 512
                    bi = tb % 4
                    bk, bkey = bank(bi)
                    for gi in range(4):
                        P.op(PE, lambda e, gi=gi, bk=bk, c0=c0: e.matmul(bk[:], Cpad[:, 4 * q + gi, :], Xs[gi][:, c0:c0 + 512], start=(gi == 0), stop=(gi == 3)),
                             reads=["Cpad", ("Xg", gi)] + [("X", gi, b_) for b_ in range(16)], writes=[bkey])
                    ys = ycnt % 2; ycnt += 1
                    yk, tk, zk = ("yv", ys), ("ta", ys), ("zs", ys)
                    P.op(DVE, lambda e, bk=bk, c0=c0, ys=ys: e.scalar_tensor_tensor(out=yv[:, ys, :], in0=UTc[:, c0:c0 + 512].bitcast(F32), scalar=Dpad[:, q:q + 1], in1=bk[:], op0=ALU.mult, op1=ALU.add),
                         reads=["UTc", "Dpad", bkey] + [("UTc", g_) for g_ in range(4)], writes=[yk])
                    P.op(POOL, lambda e, ys=ys: e.tensor_tensor(out=ta[:, ys, :], in0=yv[:, ys, :], in1=yv[:, ys, :], op=ALU.mult), reads=[yk], writes=[tk])
                    P.op(POOL, lambda e, ys=ys: e.tensor_scalar(out=ta[:, ys, :], in0=ta[:, ys, :], scalar1=0.044715, scalar2=1.0, op0=ALU.mult, op1=ALU.add), reads=[tk], writes=[tk])
                    P.op(POOL, lambda e, ys=ys: e.tensor_tensor(out=ta[:, ys, :], in0=ta[:, ys, :], in1=yv[:, ys, :], op=ALU.mult), reads=[tk, yk], writes=[tk])
                    P.op(ACT, lambda e, ys=ys: e.activation(out=ta[:, ys, :], in_=ta[:, ys, :], func=AF.Sigmoid, scale=1.5957691216), reads=[tk], writes=[tk])
                    P.op(POOL, lambda e, ys=ys: e.tensor_tensor(out=zs[:, ys, :], in0=ta[:, ys, :], in1=yv[:, ys, :], op=ALU.mult), reads=[tk, yk], writes=[tk])
                    P.dma(ACT, lambda e, ys=ys, tb=tb: e.dma_start(out=ZT[q, :, tb * 512:(tb + 1) * 512], in_=zs[:, ys, :]), reads=[tk], writes=[("ZT", q, tb)])
        P.barrier()
        sst.close()
        if stop <= 2:
            P.emit(); return nc

        try:
            wdense_cm = contextlib.ExitStack()
            wts = sb(wdense_cm, "wts", [128, 16, 2]); idxi = sb(wdense_cm, "idxi", [128, 32], mybir.dt.int32)
            with contextlib.ExitStack() as st:
                KT = sb(st, "KT", [128, 4, 640], F32R)
                VA = sb(st, "VA", [128, 17, 132], F32R)
                mcur = sb(st, "mcur", [128, 512], F32R); mprev = sb(st, "mprev", [128, 512], F32R); mprev0 = sb(st, "mprev0", [128, 512], F32R)
                wload(mcur[:], mcur_d, "mcur"); wload(mprev[:], mprev_d, "mprev"); wload(mprev0[:], mprev0_d, "mprev0")
                sinke = sb(st, "sinke", [128, 8]); bg = sb(st, "bg", [128, 16]); bgl = sb(st, "bgl", [128, 8])
                P.dma(SP, lambda e: e.dma_start(out=sinke[:], in_=sinks), writes=["sinke"])
                P.op(ACT, lambda e: e.activation(out=sinke[:], in_=sinke[:], func=AF.Exp), reads=["sinke"], writes=["sinke"])
                P.dma(SP, lambda e: e.dma_start(out=bg[:], in_=bgate), writes=["bg"])
                P.dma(SP, lambda e: e.dma_start(out=bgl[:], in_=bglu), writes=["bgl"])
                i17 = ident[:, 0:17].rearrange("p (a b) -> p a b", b=1)
                P.op(POOL, lambda e: e.tensor_scalar(out=VA[:, :, 64:65], in0=i17, scalar1=0.0, scalar2=1.0, op0=ALU.mult, op1=ALU.add), reads=["ident"], writes=["VAones"])
                P.op(POOL, lambda e: e.tensor_scalar(out=VA[:, :, 130:131], in0=i17, scalar1=0.0, scalar2=1.0, op0=ALU.mult, op1=ALU.add), reads=["ident"], writes=["VAones2"])
                P.op(POOL, lambda e: e.tensor_scalar(out=VA[:, :, 65:66], in0=i17, scalar1=0.0, scalar2=None, op0=ALU.mult), reads=["ident", "VAones"], writes=["VAones"])
                P.op(POOL, lambda e: e.tensor_scalar(out=VA[:, :, 131:132], in0=i17, scalar1=0.0, scalar2=None, op0=ALU.mult), reads=["ident", "VAones2"], writes=["VAones2"])
                gmoef = make_gfull(st, "gmoe", gmoe)
                wrt = sb(st, "wrt", [128, 8, 36]); brt = sb(st, "brt", [128, 36])
                P.dma(SP, lambda e: e.dma_start(out=wrt[:], in_=wr), writes=["wrt"])
                P.dma(SP, lambda e: e.dma_start(out=brt[:], in_=br), writes=["brt"])
                ring = [sb(st, "ring%d" % i, [128, 4096], F32R) for i in range(4)]
                rc = [0]

                def wpanel(src_ap, shape_str, k):
                    i = rc[0] % 4; rc[0] += 1
                    c = src_ap.shape[-1]
                    v = ring[i][:, 0:k * c].rearrange(shape_str, k=k)
                    wload(v, src_ap, ("ring", i))
                    return v, ("ring", i)

                w_in_v = w_in.rearrange("(k p) c -> p k c", p=128)
                hT = sb(st, "hT", [128, 8, 512], F32R)
                qp = sb(st, "qp", [128, 8, 512], F32R)
                QT = qp[:, 0:4, :]
                PT = qp[:, 4:8, :].rearrange("p (a b) t -> p a b t", a=2)
                atok = sb(st, "atok", [128, 512]); rden = sb(st, "rden", [128, 8])
                aT = sb(st, "aT", [128, 4, 512], F32R)
                zT = sb(st, "zT", [128, 8, 512], F32R); z2T = sb(st, "z2T", [128, 8, 512], F32R)
                sg = sb(st, "sg", [128, 2, 512]); mt = sb(st, "mt", [128, 2, 512])
                mgT = qp
                xt = sb(st, "b_xt", [128, 2, 1024]); xn = sb(st, "b_xn", [128, 2, 1024]); junk = 2
                ustr = sb(st, "ustr", [128, 128]); ones_t = sb(st, "ones_t", [128, 128]); ecap = sb(st, "ecap", [128, 32]); cntb = sb(st, "cntb", [128, 32])
                Am = sb(st, "Am", [128, 32]); posf = sb(st, "posf", [128, 32]); ptmp = sb(st, "ptmp", [128, 32]); idxf = sb(st, "idxf", [128, 4])
                P.dma(SP, lambda e: e.dma_start(out=ustr[:], in_=ustr_d), writes=["ustr"])
                P.dma(SP, lambda e: e.dma_start(out=ones_t[:], in_=ones_d), writes=["ones_t"])
                P.dma(SP, lambda e: e.dma_start(out=ecap[:], in_=ecap_d), writes=["ecap"])
                P.op(DVE, lambda e: e.memset(cntb[:], 0.0), writes=["cntb"])
                ss = sb(st, "b_ss", [128, 16]); rs = sb(st, "b_rs", [128, 16])
                h2g = hT
                lg = sb(st, "lg", [128, 36]); rt = {nm: sb(st, "rt_" + nm, [128, 40]) for nm in ["a", "b", "c", "d", "e", "f", "g", "h"]}
                wload(hT[:, :, 0:128], HT[:, :, 0:128].rearrange("k p t -> p k t"), "hT", reads=[("HT", 0)])

                def proj_fm(wv, wkey, ncol_chunks, rhs_fn, rkey, N, out_fn, okeys_fn, evac_scale=None, nk=8):
                    for ch in range(ncol_chunks):
                        bi = pcnt[0] % 4; pcnt[0] += 1
                        bk, bkey = bank(bi)
                        for k in range(nk):
                            P.op(PE, lambda e, k=k, ch=ch, bk=bk: e.matmul(bk[:, 0:N], wv[:, k, ch * 128:(ch + 1) * 128], rhs_fn(k), start=(k == 0), stop=(k == nk - 1)),
                                 reads=[wkey, rkey], writes=[bkey])
                        if evac_scale is None:
                            P.op(ACT, lambda e, ch=ch, bk=bk: e.copy(out=out_fn(ch), in_=bk[:, 0:N]), reads=[bkey], writes=okeys_fn(ch))
                        else:
                            P.op(ACT, lambda e, ch=ch, bk=bk: e.mul(out=out_fn(ch), in_=bk[:, 0:N], mul=evac_scale), reads=[bkey], writes=okeys_fn(ch))

                def kv_proj(hsrc, hkey, N, col0, tiles):
                    wv, wk = wpanel(w_in_v[:, :, CK:CK + 512], "p (k c) -> p k c", k=8)
                    proj_fm(wv, wk, 4, lambda k: hsrc[:, k, 0:N], hkey, N, lambda ch: KT[:, ch, col0:col0 + N], lambda ch: [("KT", ch)])
                    wv, wk = wpanel(w_in_v[:, :, CV:CV + 128], "p (k c) -> p k c", k=8)
                    for ti, tile_idx in enumerate(tiles):
                        bi = pcnt[0] % 4; pcnt[0] += 1
                        bk, bkey = bank(bi)
                        for k in range(8):
                            P.op(PE, lambda e, k=k, ti=ti, bk=bk: e.matmul(bk[:, 0:128], hsrc[:, k, ti * 128:(ti + 1) * 128], wv[:, k, :], start=(k == 0), stop=(k == 7)),
                                 reads=[wk, hkey], writes=[bkey])
                        P.op(ACT, lambda e, bk=bk, tile_idx=tile_idx: e.copy(out=VA[:, tile_idx, 0:64], in_=bk[:, 0:64]), reads=[bkey], writes=[("VA", tile_idx, 0)])
                        P.op(ACT, lambda e, bk=bk, tile_idx=tile_idx: e.copy(out=VA[:, tile_idx, 66:130], in_=bk[:, 64:128]), reads=[bkey], writes=[("VA", tile_idx, 1)])

                kv_proj(hT, "hT", 128, 0, [0])
                if stop <= 2.2:
                    st.close()
                    raise _Stop

                for tg in range(4):
                    wload(hT[:], HT[:, :, 128 + tg * 512:128 + (tg + 1) * 512].rearrange("k p t -> p k t"), "hT", reads=[("HT", tg + 1)])
                    wload(zT[:], ZT[:, :, tg * 512:(tg + 1) * 512].rearrange("k p t -> p k t"), "zT", reads=[("ZT", q_, tg) for q_ in range(8)])
                    wv, wk = wpanel(w_in_v[:, :, CQ:CQ + 512], "p (k c) -> p k c", k=8)
                    proj_fm(wv, wk, 4, lambda k: hT[:, k, :], "hT", 512, lambda ch: QT[:, ch, :], lambda ch: [("qp", ch)], evac_scale=0.125)
                    if tg > 0:
                        P.op(DVE, lambda e: e.tensor_copy(out=KT[:, :, 0:128], in_=KT[:, :, 512:640]), reads=[("KT", c_) for c_ in range(4)], writes=[("KT", c_) for c_ in range(4)])
                    kv_proj(hT, "hT", 512, 128, [1 + tg * 4 + j for j in range(4)])
                    if stop <= 2.4 and tg == DBG_TG:
                        st.close()
                        raise _Stop
                    for qb in range(4):
                        blk = 1 + tg * 4 + qb
                        obanks = []
                        for kv in range(2):
                            for kbi, kb in enumerate((blk - 1, blk)):
                                bi = 4 + (pcnt[0] % 2); pcnt[0] += 1
                                bk, bkey = bank(bi)
                                if kbi == 1:
                                    mk, mkey = mcur, "mcur"
                                elif blk == 1:
                                    mk, mkey = mprev0, "mprev0"
                                else:
                                    mk, mkey = mprev, "mprev"
                                P.op(PE, lambda e, bk=bk, mk=mk: e.matmul(bk[:], identr[:], mk[:], start=True, stop=False), reads=["identr", mkey], writes=[bkey])
                                for hh in range(4):
                                    h = kv * 4 + hh
                                    half = h % 2
                                    P.op(PE, lambda e, bk=bk, hh=hh, h=h, half=half, kb=kb, kv=kv, qb=qb, kbi=kbi: e.matmul(
                                        bk[:, hh * 128:(hh + 1) * 128], KT[:, kv * 2 + half, (qb + kbi) * 128:(qb + kbi + 1) * 128],
                                        QT[:, h // 2, qb * 128:(qb + 1) * 128], start=False, stop=(hh == 3)),
                                        reads=[("KT", kv * 2 + half), ("qp", h // 2)], writes=[bkey])
                                P.op(ACT, lambda e, bk=bk, kv=kv, kbi=kbi: e.activation(out=PT[:, kv, kbi, :], in_=bk[:], func=AF.Exp), reads=[bkey], writes=[("qp", 4 + kv * 2 + kbi)])
                            if stop <= 2.45 and tg == DBG_TG:
                                st.close()
                                raise _Stop
                            ob = 6 + kv
                            bko, bokey = bank(ob)
                            for hh in range(4):
                                for kbi, kb in enumerate((blk - 1, blk)):
                                    P.op(PE, lambda e, bko=bko, hh=hh, kbi=kbi, kb=kb, kv=kv: e.matmul(
                                        bko[:, hh * 66:(hh + 1) * 66], PT[:, kv, kbi, hh * 128:(hh + 1) * 128], VA[:, kb, kv * 66:(kv + 1) * 66],
                                        start=(kbi == 0), stop=(kbi == 1)),
                                        reads=[("qp", 4 + kv * 2 + kbi), ("VA", kb, kv), "VAones", "VAones2"], writes=[bokey])
                            if stop <= 2.5 and tg == DBG_TG:
                                st.close()
                                raise _Stop
                            ov = bko[:, 0:264].rearrange("p (h d) -> p h d", h=4)
                            P.op(DVE, lambda e, ov=ov, kv=kv: e.tensor_tensor(out=rden[:, kv * 4:(kv + 1) * 4], in0=ov[:, :, 64], in1=sinke[:, kv * 4:(kv + 1) * 4], op=ALU.add),
                                 reads=[bokey, "sinke"], writes=[("rden", kv)])
                            P.op(DVE, lambda e, kv=kv: e.reciprocal(out=rden[:, kv * 4:(kv + 1) * 4], in_=rden[:, kv * 4:(kv + 1) * 4]), reads=[("rden", kv)], writes=[("rden", kv)])
                            for hh in range(4):
                                h = kv * 4 + hh
                                P.op(DVE, lambda e, ov=ov, hh=hh, h=h: e.tensor_scalar(out=atok[:, h * 64:(h + 1) * 64], in0=ov[:, hh, 0:64], scalar1=rden[:, h:h + 1], scalar2=None, op0=ALU.mult),
                                     reads=[bokey, ("rden", kv)], writes=[("atok", kv)])
                        if stop <= 2.55 and tg == DBG_TG:
                            st.close()
                            raise _Stop
                        bi = pcnt[0] % 4; pcnt[0] += 1
                        bk, bkey = bank(bi)
                        for j in range(4):
                            P.op(PE, lambda e, j=j, bk=bk: e.transpose(out=bk[:, j * 128:(j + 1) * 128], in_=atok[:, j * 128:(j + 1) * 128], identity=ident[:]),
                                 reads=[("atok", 0), ("atok", 1), "ident"], writes=[bkey])
                        P.op(ACT, lambda e, bk=bk, qb=qb: e.copy(out=aT[:, :, qb * 128:(qb + 1) * 128], in_=bk[:].rearrange("p (k t) -> p k t", k=4)), reads=[bkey], writes=[("aT", qb)])
                    aTk = [("aT", qb) for qb in range(4)]
                    if stop <= 2.6 and tg == DBG_TG:
                        st.close()
                        raise _Stop
                    for hf in range(2):
                        wv, wk = wpanel(wglu.rearrange("(k p) c -> p k c", p=128)[:, :, hf * 512:(hf + 1) * 512], "p (k c) -> p k c", k=8)
                        for c4 in range(4):
                            ch = hf * 4 + c4
                            bi = pcnt[0] % 4; pcnt[0] += 1
                            bk, bkey = bank(bi)
                            for k in range(8):
                                P.op(PE, lambda e, k=k, c4=c4, bk=bk: e.matmul(bk[:], wv[:, k, c4 * 128:(c4 + 1) * 128], zT[:, k, :], start=(k == 0), stop=(k == 7)), reads=[wk, "zT"], writes=[bkey])
                            s_ = ch % 2
                            P.op(ACT, lambda e, ch=ch, bk=bk, s_=s_: e.activation(out=sg[:, s_, :], in_=bk[:], func=AF.Sigmoid, bias=bgl[:, ch:ch + 1]), reads=[bkey, "bgl"], writes=[("sg", s_)])
                            P.op(POOL, lambda e, ch=ch, s_=s_: e.tensor_tensor(out=z2T[:, ch, :], in0=sg[:, s_, :], in1=zT[:, ch, :].bitcast(F32), op=ALU.mult), reads=[("sg", s_), "zT"], writes=[("z2T", ch)])
                    z2k = [("z2T", ch) for ch in range(8)]
                    for hf in range(2):
                        wab_v, wab_k = wpanel(wab.rearrange("(k p) c -> p k c", p=128)[:, :, hf * 512:(hf + 1) * 512], "p (k c) -> p k c", k=4)
                        wsb_v, wsb_k = wpanel(wsb.rearrange("(k p) c -> p k c", p=128)[:, :, hf * 512:(hf + 1) * 512], "p (k c) -> p k c", k=8)
                        wga_v, wga_k = wpanel(w_in_v[:, :, CG + hf * 512:CG + (hf + 1) * 512], "p (k c) -> p k c", k=8)
                        wgs_v, wgs_k = wpanel(w_in_v[:, :, CG + 1024 + hf * 512:CG + 1024 + (hf + 1) * 512], "p (k c) -> p k c", k=8)
                        for c4 in range(4):
                            dmc = hf * 4 + c4
                            specs = [(wga_v, wga_k, 8, lambda k: hT[:, k, :], ["hT"]), (wab_v, wab_k, 4, lambda k: aT[:, k, :], aTk),
                                     (wgs_v, wgs_k, 8, lambda k: hT[:, k, :], ["hT"]), (wsb_v, wsb_k, 8, lambda k: z2T[:, k, :], z2k)]
                            bks = []
                            for si, (wv_, wk_, nk, rf, rk) in enumerate(specs):
                                bi = si
                                bk, bkey = bank(bi)
                                bks.append((bk, bkey))
                                for k in range(nk):
                                    P.op(PE, lambda e, k=k, c4=c4, bk=bk, wv_=wv_, rf=rf, nk=nk: e.matmul(bk[:], wv_[:, k, c4 * 128:(c4 + 1) * 128], rf(k), start=(k == 0), stop=(k == nk - 1)),
                                         reads=[wk_] + rk, writes=[bkey])
                            for br_i in range(2):
                                (gk, gkey), (vk, vkey) = bks[2 * br_i], bks[2 * br_i + 1]
                                bcol = br_i * 8 + dmc
                                P.op(ACT, lambda e, gk=gk, bcol=bcol, br_i=br_i: e.activation(out=sg[:, br_i, :], in_=gk[:], func=AF.Sigmoid, bias=bg[:, bcol:bcol + 1]), reads=[gkey, "bg"], writes=[("sg", br_i)])
                                P.op(DVE, lambda e, vk=vk, br_i=br_i: e.tensor_tensor(out=mt[:, br_i, :], in0=vk[:], in1=sg[:, br_i, :], op=ALU.mult), reads=[vkey, ("sg", br_i)], writes=[("mt", br_i)])
                            P.op(POOL, lambda e, dmc=dmc: e.tensor_tensor(out=mgT[:, dmc, :], in0=mt[:, 0, :], in1=mt[:, 1, :], op=ALU.add), reads=[("mt", 0), ("mt", 1)], writes=[("qp", dmc)])
                    mgk = [("qp", c_) for c_ in range(8)]
                    if stop <= 2.8 and tg == DBG_TG:
                        st.close()
                        raise _Stop
                    wo = []
                    for hf in range(2):
                        wo.append(wpanel(wout.rearrange("(k p) c -> p k c", p=128)[:, :, hf * 512:(hf + 1) * 512], "p (k c) -> p k c", k=8))
                    for j in range(4):
                        ti = tg * 4 + j
                        s = ti % 2
                        xk = ("b_xt", s)
                        P.dma(SP, lambda e, ti=ti, s=s: e.dma_start(out=xt[:, s, :], in_=x_all[6144 + ti * 128:6144 + (ti + 1) * 128, :]), writes=[xk])
                        for hf in range(2):
                            bi = 4 + hf
                            bk, bkey = bank(bi)
                            for k in range(8):
                                P.op(PE, lambda e, k=k, hf=hf, bk=bk, j=j: e.matmul(bk[:], mgT[:, k, j * 128:(j + 1) * 128], wo[hf][0][:, k, :], start=(k == 0), stop=(k == 7)),
                                     reads=mgk + [wo[hf][1]], writes=[bkey])
                            P.op(DVE, lambda e, hf=hf, bk=bk, s=s: e.tensor_tensor(out=xt[:, s, hf * 512:(hf + 1) * 512], in0=bk[:], in1=xt[:, s, hf * 512:(hf + 1) * 512], op=ALU.add),
                                 reads=[bkey, xk], writes=[xk])
                        for hf in range(2):
                            P.dma(ACT, lambda e, ti=ti, s=s, hf=hf: e.dma_start(out=X1[ti * 128:(ti + 1) * 128, hf * 512:(hf + 1) * 512], in_=xt[:, s, hf * 512:(hf + 1) * 512]), reads=[xk], writes=[("X1", ti, hf)])
                        if stop <= 2.9 and tg == DBG_TG:
                            st.close()
                            raise _Stop
                        norm_T((xt, xn, junk, ss, rs), None, ti, gmoef, "gmoe", h2g, "hT", j * 128, "b_")
                        if stop <= 2.95 and tg == DBG_TG:
                            st.close()
                            raise _Stop
                        bk, bkey = bank(6)
                        for k in range(8):
                            P.op(PE, lambda e, k=k, bk=bk, j=j: e.matmul(bk[:, 0:36], h2g[:, k, j * 128:(j + 1) * 128].bitcast(F32), wrt[:, k, :], start=(k == 0), stop=(k == 7)),
                                 reads=["hT", "wrt"], writes=[bkey])
                        R = lambda nm: rt[nm]
                        D = lambda fn, rd, wr_: P.op(DVE, fn, reads=rd, writes=wr_)
                        D(lambda e, bk=bk: e.tensor_tensor(out=lg[:], in0=bk[:, 0:36], in1=brt[:], op=ALU.add), [bkey, "brt"], ["lg"])
                        if stop <= 2.96 and tg == DBG_TG:
                            st.close()
                            raise _Stop
                        D(lambda e: e.tensor_reduce(out=R("a")[:, 0:1], in_=lg[:, 0:4], axis=AX.X, op=ALU.max), ["lg"], ["ra"])
                        D(lambda e: e.tensor_scalar(out=R("b")[:, 0:4], in0=lg[:, 0:4], scalar1=R("a")[:, 0:1], scalar2=None, op0=ALU.subtract), ["lg", "ra"], ["rb"])
                        P.op(ACT, lambda e: e.activation(out=R("b")[:, 0:4], in_=R("b")[:, 0:4], func=AF.Exp, accum_out=R("a")[:, 1:2]), reads=["rb"], writes=["rb", "ra2"])
                        D(lambda e: e.reciprocal(out=R("a")[:, 2:3], in_=R("a")[:, 1:2]), ["ra2"], ["gp"])
                        D(lambda e: e.tensor_scalar(out=R("c")[:, 0:4], in0=lg[:, 0:4], scalar1=R("a")[:, 0:1], scalar2=None, op0=ALU.is_ge), ["lg", "ra"], ["rc"])
                        if stop <= 2.97 and tg == DBG_TG:
                            st.close()
                            raise _Stop
                        D(lambda e: e.tensor_scalar(out=R("d")[:, 0:4], in0=R("c")[:, 0:4], scalar1=-1.0, scalar2=1.0e4, op0=ALU.add, op1=ALU.mult), ["rc"], ["rd"])
                        lg3 = lg[:, 4:36].rearrange("p (g j) -> p g j", g=4)
                        e3 = R("e")[:, 0:32].rearrange("p (g j) -> p g j", g=4)
                        for g_ in range(4):
                            D(lambda e, g_=g_: e.tensor_scalar(out=e3[:, g_, :], in0=lg3[:, g_, :], scalar1=R("d")[:, g_:g_ + 1], scalar2=None, op0=ALU.add), ["lg", "rd"], ["re"])
                        D(lambda e: e.tensor_reduce(out=R("f")[:, 0:1], in_=R("e")[:, 0:32], axis=AX.X, op=ALU.max), ["re"], ["m1"])
                        D(lambda e: e.tensor_scalar(out=R("g")[:, 0:32], in0=R("e")[:, 0:32], scalar1=R("f")[:, 0:1], scalar2=None, op0=ALU.is_ge), ["re", "m1"], ["oh1"])
                        D(lambda e: e.scalar_tensor_tensor(out=R("h")[:, 0:32], in0=R("g")[:, 0:32], scalar=-1.0e4, in1=R("e")[:, 0:32], op0=ALU.mult, op1=ALU.add), ["oh1", "re"], ["rh"])
                        D(lambda e: e.tensor_reduce(out=R("f")[:, 1:2], in_=R("h")[:, 0:32], axis=AX.X, op=ALU.max), ["rh"], ["m2"])
                        D(lambda e: e.tensor_scalar(out=R("h")[:, 0:32], in0=R("h")[:, 0:32], scalar1=R("f")[:, 1:2], scalar2=None, op0=ALU.is_ge), ["rh", "m2"], ["oh2"])
                        D(lambda e: e.tensor_tensor(out=R("f")[:, 2:3], in0=R("f")[:, 1:2], in1=R("f")[:, 0:1], op=ALU.subtract), ["m1", "m2"], ["df"])
                        P.op(ACT, lambda e: e.activation(out=R("f")[:, 2:3], in_=R("f")[:, 2:3], func=AF.Exp), reads=["df"], writes=["df"])
                        D(lambda e: e.tensor_scalar(out=R("f")[:, 2:3], in0=R("f")[:, 2:3], scalar1=1.0, scalar2=None, op0=ALU.add), ["df"], ["df"])
                        D(lambda e: e.reciprocal(out=R("f")[:, 3:4], in_=R("f")[:, 2:3]), ["df"], ["w1"])
                        D(lambda e: e.tensor_tensor(out=R("f")[:, 3:4], in0=R("f")[:, 3:4], in1=R("a")[:, 2:3], op=ALU.mult), ["w1", "gp"], ["w1"])
                        D(lambda e: e.tensor_tensor(out=R("f")[:, 4:5], in0=R("a")[:, 2:3], in1=R("f")[:, 3:4], op=ALU.subtract), ["w1", "gp"], ["w2"])
                        D(lambda e, ti=ti: e.tensor_copy(out=wts[:, ti, 0:1], in_=R("f")[:, 3:4]), ["w1"], [("wts", ti)])
                        D(lambda e, ti=ti: e.tensor_copy(out=wts[:, ti, 1:2], in_=R("f")[:, 4:5]), ["w2", ("wts", ti)], [("wts", ti)])
                        D(lambda e: e.tensor_tensor(out=Am[:], in0=R("g")[:, 0:32], in1=R("h")[:, 0:32], op=ALU.add), ["oh1", "oh2"], ["Am"])
                        bkp, bkpkey = bank(7)
                        P.op(PE, lambda e, bkp=bkp: e.matmul(bkp[:, 0:32], ustr[:], Am[:], start=True, stop=True), reads=["ustr", "Am"], writes=[bkpkey])
                        P.op(PE, lambda e, bkp=bkp: e.matmul(bkp[:, 32:64], ones_t[:], Am[:], start=True, stop=True), reads=["ones_t", "Am"], writes=[bkpkey])
                        D(lambda e, bkp=bkp: e.tensor_tensor(out=posf[:], in0=bkp[:, 0:32], in1=cntb[:], op=ALU.add), [bkpkey, "cntb"], ["posf"])
                        D(lambda e, bkp=bkp: e.tensor_tensor(out=cntb[:], in0=bkp[:, 32:64], in1=cntb[:], op=ALU.add), [bkpkey, "cntb", "posf"], ["cntb"])
                        for kk, ohn in ((0, "g"), (1, "h")):
                            okey = "oh1" if kk == 0 else "oh2"
                            D(lambda e, ohn=ohn: e.tensor_tensor(out=ptmp[:], in0=R(ohn)[:, 0:32], in1=posf[:], op=ALU.mult), [okey, "posf"], ["ptmp"])
                            D(lambda e, kk=kk: e.tensor_reduce(out=idxf[:, 2 + kk:3 + kk], in_=ptmp[:], axis=AX.X, op=ALU.add), ["ptmp"], [("idxf", kk)])
                            D(lambda e, ohn=ohn: e.tensor_tensor(out=ptmp[:], in0=R(ohn)[:, 0:32], in1=ecap[:], op=ALU.mult), [okey, "ecap", ("idxf", kk)], ["ptmp"])
                            D(lambda e, kk=kk: e.tensor_reduce(out=idxf[:, kk:kk + 1], in_=ptmp[:], axis=AX.X, op=ALU.add), ["ptmp"], [("idxf", kk)])
                            D(lambda e, kk=kk: e.tensor_tensor(out=idxf[:, kk:kk + 1], in0=idxf[:, kk:kk + 1], in1=idxf[:, 2 + kk:3 + kk], op=ALU.add), [("idxf", kk)], [("idxf", kk)])
                            D(lambda e, kk=kk: e.tensor_scalar(out=idxf[:, 2 + kk:3 + kk], in0=idxf[:, 2 + kk:3 + kk], scalar1=float(CAP) - 0.5, scalar2=1.0e6, op0=ALU.is_gt, op1=ALU.mult), [("idxf", kk)], [("idxf", kk)])
                            D(lambda e, kk=kk: e.tensor_tensor(out=idxf[:, kk:kk + 1], in0=idxf[:, kk:kk + 1], in1=idxf[:, 2 + kk:3 + kk], op=ALU.add), [("idxf", kk)], [("idxf", kk)])
                            D(lambda e, kk=kk, ti=ti: e.tensor_copy(out=idxi[:, ti * 2 + kk:ti * 2 + kk + 1], in_=idxf[:, kk:kk + 1]), [("idxf", kk)], [("idxi", ti, kk)])
                            P.dma(POOL, lambda e, kk=kk, ti=ti: e.indirect_dma_start(out=Xscr[:, :], out_offset=bass.IndirectOffsetOnAxis(ap=idxi[:, ti * 2 + kk:ti * 2 + kk + 1], axis=0),
                                                                                     in_=xn[:, ti % 2, :], in_offset=None, bounds_check=NROW - 1, oob_is_err=False),
                                  reads=[("idxi", ti, kk), ("b_xn", ti % 2)] + [("Xz", r_) for r_ in range(0, 64, 2)], writes=[("Xs", ti, kk)])
                        if stop <= 2.98 and tg == DBG_TG:
                            st.close()
                            raise _Stop
                    if stop <= 2.99 and tg == DBG_TG:
                        st.close()
                        raise _Stop
                    if stop <= 2.995 and tg == DBG_TG:
                        st.close()
                        raise _Stop
        except _Stop:
            pass
        P.barrier()
        if debug and stop == 3:
            dX1 = nc.dram_tensor("dX1", [NOWN, 1024], F32, kind="ExternalOutput").ap()
            dIdx = nc.dram_tensor("dIdx", [128, 32], mybir.dt.int32, kind="ExternalOutput").ap()
            dWD = nc.dram_tensor("dWD", [128, 16, 2], F32, kind="ExternalOutput").ap()
            dXs = nc.dram_tensor("dXs", [NROW, 1024], F32, kind="ExternalOutput").ap()
            for i_ in range(4):
                P.dma(SP, lambda e, i_=i_: e.dma_start(out=dX1[i_ * 512:(i_ + 1) * 512, :], in_=X1[i_ * 512:(i_ + 1) * 512, :]), writes=[("dX1", i_)])
            for i_ in range(8):
                P.dma(SP, lambda e, i_=i_: e.dma_start(out=dXs[i_ * 1024:(i_ + 1) * 1024, :], in_=Xscr[i_ * 1024:(i_ + 1) * 1024, :]), writes=[("dXs", i_)])
            P.dma(SP, lambda e: e.dma_start(out=dWD, in_=wts[:]), writes=["dWD"])
            P.dma(SP, lambda e: e.dma_start(out=dIdx, in_=idxi[:]), writes=["dIdx"])
            P.barrier()
        if stop <= 3:
            wdense_cm.close(); P.emit(); return nc

        with contextlib.ExitStack() as st:
            gmoef = make_gfull(st, "gmoe4", gmoe)
            wg = [sb(st, "wg%d" % i, [128, 8, 512], F32R) for i in range(2)]
            wu = [sb(st, "wu%d" % i, [128, 8, 512], F32R) for i in range(2)]
            wd = [sb(st, "wd%d" % i, [128, 4, 1024], F32R) for i in range(2)]
            Xr = sb(st, "Xr", [128, 2, 2, 1024]); XeT = sb(st, "XeT", [128, 2, 8, CAP], F32R)
            aTm = sb(st, "aTm", [128, 2, 4, CAP], F32R); sil = sb(st, "sil", [128, 2, CAP])
            Yt = sb(st, "Yt", [128, 2, 2, 1024])
            gf = sb(st, "gf", [128, 1024]); xo = sb(st, "xo", [128, 2, 1024]); ygl = [[sb(st, "yg%d%d" % (a_, b_), [128, 1024]) for b_ in range(2)] for a_ in range(2)]
            fss = sb(st, "fss", [128, 16])
            P.dma(SP, lambda e: e.dma_start(out=gf[:], in_=gfin), writes=["gf"])
            allXs = [("Xs", ti, kk) for ti in range(16) for kk in range(2)]
            for ex in range(32):
                s = ex % 2
                wload(wg[s][:], weg[ex].rearrange("(k p) c -> p k c", p=128), ("wg", s))
                wload(wu[s][:], weu[ex].rearrange("(k p) c -> p k c", p=128), ("wu", s))
                wload(wd[s][:], wed[ex].rearrange("(k p) c -> p k c", p=128), ("wd", s))
                P.dma(SP, lambda e, ex=ex, s=s: e.dma_start(out=Xr[:, s], in_=Xscr[ex * CAP:(ex + 1) * CAP, :].rearrange("(rt p) c -> p rt c", p=128)),
                      reads=allXs, writes=[("Xr", s)])
                for rt in range(2):
                    for hh in range(2):
                        bi = pcnt[0] % 2; pcnt[0] += 1
                        bk, bkey = bank(bi)
                        for j in range(4):
                            k = hh * 4 + j
                            P.op(PE, lambda e, k=k, j=j, bk=bk, rt=rt, s=s: e.transpose(out=bk[:, j * 128:(j + 1) * 128], in_=Xr[:, s, rt, k * 128:(k + 1) * 128], identity=ident[:]),
                                 reads=[("Xr", s), "ident"], writes=[bkey])
                        P.op(DVE, lambda e, hh=hh, bk=bk, rt=rt, s=s: e.tensor_tensor(out=XeT[:, s, hh * 4:hh * 4 + 4, rt * 128:(rt + 1) * 128],
                                                                                   in0=bk[:].rearrange("p (k t) -> p k t", k=4), in1=gmoef[:, hh * 4:hh * 4 + 4, :], op=ALU.mult),
                             reads=[bkey, "gmoe4"], writes=[("XeT", s, rt, hh)])
                xek = [("XeT", s, rt, hh) for rt in range(2) for hh in range(2)]
                for fc in range(4):
                    pr = 2 + (pcnt[0] % 2) * 2; pcnt[0] += 1
                    (bg_, bgk), (bu_, buk) = bank(pr), bank(pr + 1)
                    for k in range(8):
                        P.op(PE, lambda e, k=k, fc=fc, bg_=bg_, s=s: e.matmul(bg_[:, 0:CAP], wg[s][:, k, fc * 128:(fc + 1) * 128], XeT[:, s, k, :], start=(k == 0), stop=(k == 7)),
                             reads=[("wg", s)] + xek, writes=[bgk])
                    for k in range(8):
                        P.op(PE, lambda e, k=k, fc=fc, bu_=bu_, s=s: e.matmul(bu_[:, 0:CAP], wu[s][:, k, fc * 128:(fc + 1) * 128], XeT[:, s, k, :], start=(k == 0), stop=(k == 7)),
                             reads=[("wu", s)] + xek, writes=[buk])
                    ss_ = fc % 2
                    P.op(ACT, lambda e, bg_=bg_, ss_=ss_: e.activation(out=sil[:, ss_, :], in_=bg_[:, 0:CAP], func=AF.Silu), reads=[bgk], writes=[("sil", ss_)])
                    P.op(DVE, lambda e, bu_=bu_, ss_=ss_, fc=fc, s=s: e.tensor_tensor(out=aTm[:, s, fc, :], in0=bu_[:, 0:CAP], in1=sil[:, ss_, :], op=ALU.mult),
                         reads=[buk, ("sil", ss_)], writes=[("aTm", s, fc)])
                for rt in range(2):
                    for hf in range(2):
                        bi = 6 + (pcnt[0] % 2); pcnt[0] += 1
                        bk, bkey = bank(bi)
                        for k in range(4):
                            P.op(PE, lambda e, k=k, bk=bk, rt=rt, hf=hf, s=s: e.matmul(bk[:], aTm[:, s, k, rt * 128:(rt + 1) * 128], wd[s][:, k, hf * 512:(hf + 1) * 512], start=(k == 0), stop=(k == 3)),
                                 reads=[("aTm", s, k_) for k_ in range(4)] + [("wd", s)], writes=[bkey])
                        P.op(ACT, lambda e, bk=bk, rt=rt, hf=hf, s=s: e.copy(out=Yt[:, s, rt, hf * 512:(hf + 1) * 512], in_=bk[:]), reads=[bkey], writes=[("Yt", s, rt, hf)])
                P.dma(ACT, lambda e, ex=ex, s=s: e.dma_start(out=Yscr[ex * CAP:(ex + 1) * CAP, :].rearrange("(rt p) c -> p rt c", p=128), in_=Yt[:, s]),
                      reads=[("Yt", s, rt, hf) for rt in range(2) for hf in range(2)], writes=[("Ys", ex)])
            allYs = [("Ys", ex) for ex in range(32)]
            for ti in range(16):
                s = ti % 2
                xk = ("xo", s)
                P.dma(SP, lambda e, ti=ti, s=s: e.dma_start(out=xo[:, s, :], in_=X1[ti * 128:(ti + 1) * 128, :]), reads=[("X1", ti, 0), ("X1", ti, 1)], writes=[xk])
                for kk in range(2):
                    P.op(DVE, lambda e, s=s, kk=kk: e.memset(ygl[s][kk][:, :], 0.0), writes=[("yg", s, kk)])
                    P.dma(POOL, lambda e, ti=ti, s=s, kk=kk: e.indirect_dma_start(out=ygl[s][kk][:, :], out_offset=None, in_=Yscr[:, :],
                                                                                 in_offset=bass.IndirectOffsetOnAxis(ap=idxi[:, ti * 2 + kk:ti * 2 + kk + 1], axis=0), bounds_check=NROW - 1, oob_is_err=False),
                          reads=allYs + [("idxi", ti, kk), ("yg", s, kk)], writes=[("yg", s, kk)])
                    P.op(DVE, lambda e, s=s, kk=kk, ti=ti: e.scalar_tensor_tensor(out=xo[:, s, :], in0=ygl[s][kk][:, :], scalar=wts[:, ti, kk:kk + 1], in1=xo[:, s, :], op0=ALU.mult, op1=ALU.add),
                         reads=[("yg", s, kk), ("wts", ti), xk], writes=[xk])
                P.op(ACT, lambda e, s=s, ti=ti: e.activation(out=ygl[s][0][:, :], in_=xo[:, s, :], func=AF.Square, accum_out=fss[:, ti:ti + 1]), reads=[xk, ("yg", s, 0)], writes=[("yg", s, 0), ("fss", ti)])
                P.op(DVE, lambda e, ti=ti: e.tensor_scalar(out=fss[:, ti:ti + 1], in0=fss[:, ti:ti + 1], scalar1=1.0 / 1024, scalar2=EPS, op0=ALU.mult, op1=ALU.add), reads=[("fss", ti)], writes=[("fss", ti)])
                P.op(ACT, lambda e, ti=ti: e.activation(out=fss[:, ti:ti + 1], in_=fss[:, ti:ti + 1], func=AF.Sqrt), reads=[("fss", ti)], writes=[("fss", ti)])
                P.op(DVE, lambda e, ti=ti: e.reciprocal(out=fss[:, ti:ti + 1], in_=fss[:, ti:ti + 1]), reads=[("fss", ti)], writes=[("fss", ti)])
                P.op(DVE, lambda e, s=s, ti=ti: e.scalar_tensor_tensor(out=xo[:, s, :], in0=xo[:, s, :], scalar=fss[:, ti:ti + 1], in1=gf[:], op0=ALU.mult, op1=ALU.mult),
                     reads=[xk, ("fss", ti), "gf"], writes=[xk])
                P.dma(ACT, lambda e, ti=ti, s=s: e.dma_start(out=y_out[ti * 128:(ti + 1) * 128, :], in_=xo[:, s, :]), reads=[xk], writes=[("yout", ti)])
        P.barrier()
        wdense_cm.close()
        P.emit()
        global LASTP
        LASTP = P
    return nc


def _prep(inp):
    f = np.float32
    x = np.asarray(inp["x"], f)
    w_in = np.asarray(inp["w_in"], f)[0]
    wl = np.zeros((1024, WCOLS), f)
    wl[:, 0:512] = w_in[:, 0:512]
    k0, k1 = w_in[:, 512:576], w_in[:, 576:640]
    zc = np.zeros((1024, 64), f)
    wl[:, CK:CK + 512] = np.concatenate([k0, zc, zc, k0, k1, zc, zc, k1], 1)
    wl[:, CV:CV + 128] = w_in[:, 640:768]
    up = np.zeros((1024, 32, 32), f); up[:, :, :16] = w_in[:, 768:1280].reshape(1024, 32, 16)
    wl[:, CU:CU + 1024] = up.reshape(1024, 1024)
    wl[:, CG:] = w_in[:, 1280:]

    def pad_rows(w):
        o = np.zeros((32, 32, w.shape[1]), f); o[:, :16] = w.reshape(32, 16, -1); return o.reshape(1024, -1)

    def pad_vec(v):
        o = np.zeros((32, 32), f); o[:, :16] = v.reshape(32, 16); return o.reshape(1024)

    wglu = pad_rows(np.asarray(inp["w_glu"], f)[0])
    wglu = pad_rows(wglu.T.copy()).T.copy()
    bglu = pad_vec(np.asarray(inp["b_glu"], f)[0]).reshape(8, 128).T.copy()
    wsb = pad_rows(np.asarray(inp["w_ssm_branch"], f)[0])
    a_re = np.asarray(inp["ssm_a_re"], f)[0]; a_im = np.asarray(inp["ssm_a_im"], f)[0]; ldt = np.asarray(inp["ssm_log_dt"], f)[0]
    b_re = np.asarray(inp["ssm_b_re"], f)[0]; b_im = np.asarray(inp["ssm_b_im"], f)[0]
    c_re = np.asarray(inp["ssm_c_re"], f)[0]; c_im = np.asarray(inp["ssm_c_im"], f)[0]
    dup = lambda a: np.concatenate([a.T, a.T], 0).copy()
    are1, aim1 = dup(a_re), dup(a_im)
    ldt1 = np.broadcast_to(ldt[None, :], (128, 32)).copy()

    def lay2(a_gp):
        o = a_gp.reshape(8, 4, 64)
        o = np.broadcast_to(o[:, :, None, :], (8, 4, 32, 64))
        return o.transpose(1, 2, 0, 3).reshape(128, 512).copy()

    are2, aim2 = lay2(a_re), lay2(a_im)
    ldt2 = lay2(np.broadcast_to(ldt[:, None], (32, 64)))

    def lay2b(b):
        o = np.zeros((8, 4, 32, 64), f)
        o[:, :, :16, :] = b.reshape(8, 4, 64, 16).transpose(0, 1, 3, 2)
        return o.transpose(1, 2, 0, 3).reshape(128, 512).copy()

    bre2, bim2 = lay2b(b_re), lay2b(b_im)
    cst = np.concatenate([c_re.transpose(2, 0, 1), c_im.transpose(2, 0, 1)], 0).reshape(128, 512).copy()
    dpad = pad_vec(np.asarray(inp["ssm_d"], f)[0]).reshape(8, 128).T.copy()
    fm = lambda v, n: np.asarray(v, f).reshape(n, 128).T.copy()
    ident = np.eye(128, dtype=f)
    jswap = np.zeros((128, 128), f); jswap[np.arange(128), (np.arange(128) + 64) % 128] = 1
    sgn = np.ones((128, 1), f); sgn[64:] = -1
    s_ = np.arange(128)[:, None]; q_ = np.arange(128)[None, :]
    mcur = np.where(q_ >= s_, 0.0, NEG).astype(f); mprev = np.where(s_ > q_, 0.0, NEG).astype(f)
    t4 = lambda m: np.tile(m, (1, 4)).copy()
    wr = np.concatenate([np.asarray(inp["w_router_group"], f)[0], np.asarray(inp["w_router_expert"], f)[0]], 1)
    wr = wr.reshape(8, 128, 36).transpose(1, 0, 2).copy()
    brr = np.concatenate([np.asarray(inp["b_router_group"], f)[0], np.asarray(inp["b_router_expert"], f)[0]])
    common = dict(
        w_in=wl, gmix=fm(inp["norm_mix"][0], 8), gmoe=fm(inp["norm_moe"][0], 8),
        gfin=np.broadcast_to(np.asarray(inp["norm_final"], f)[None, :], (128, 1024)).copy(),
        bgate=fm(inp["b_gate"][0], 16), sinks=np.broadcast_to(np.asarray(inp["attn_sinks"], f)[0][None, :], (128, 8)).copy(),
        ident=ident, jswap=jswap, sgn=sgn, ustr=np.triu(np.ones((128, 128), f), 1), ones=np.ones((128, 128), f), ecap=np.broadcast_to((np.arange(32, dtype=f) * CAP)[None, :], (128, 32)).copy(), m3=(np.arange(128) >= 96).astype(f).reshape(128, 1), mcur=t4(mcur), mprev=t4(mprev),
        are1=are1, aim1=aim1, ldt1=ldt1, are2=are2, aim2=aim2, ldt2=ldt2, bre2=bre2, bim2=bim2, cst=cst, dpad=dpad,
        wglu=wglu, bglu=bglu, wab=np.asarray(inp["w_attn_branch"], f)[0], wsb=wsb, wout=np.asarray(inp["w_out"], f)[0],
        wr=wr, br=np.broadcast_to(brr[None, :], (128, 36)).copy(),
        weg=np.asarray(inp["w_expert_gate"], f)[0], weu=np.asarray(inp["w_expert_up"], f)[0], wed=np.asarray(inp["w_expert_down"], f)[0],
    )
    maps = []
    for c in range(8):
        b, seg = c // 4, c % 4
        xa = np.zeros((NALL, 1024), f)
        end = (seg + 1) * 2048
        xa[NALL - end:] = x[b, :end]
        m = dict(common)
        m["x_all"] = xa
        m["mprev0"] = t4(np.full((128, 128), NEG, f)) if seg == 0 else t4(mprev)
        maps.append(m)
    return maps


def kernel(**inputs):
    maps = _prep(inputs)
    nc = build_nc()
    res = run_bass_kernel_spmd(nc, maps, core_ids=list(range(8)))
    out = np.zeros((2, 8192, 1024), np.float32)
    for c in range(8):
        b, seg = c // 4, c % 4
        out[b, seg * 2048:(seg + 1) * 2048] = res.results[c]["y_out"]
    return out
```

```python
import math, contextlib, os
DBG_TG = int(os.environ.get("DBG_TG", "0"))
import numpy as np
import concourse.bass as bass
import concourse.mybir as mybir
from concourse.bass_utils import run_bass_kernel_spmd

F32 = mybir.dt.float32
F32R = mybir.dt.float32r
AF = mybir.ActivationFunctionType
ALU = mybir.AluOpType
AX = mybir.AxisListType

PE, ACT, DVE, POOL, SP = "pe", "act", "dve", "pool", "sp"
ENGS = [PE, ACT, DVE, POOL, SP]
NDSEM = 14
EPS = 1e-6
NT = 64
NOWN = 2048
NALL = 8192
WCOLS = 512 + 512 + 128 + 1024 + 2048
CQ, CK, CV, CU, CG = 0, 512, 1024, 1152, 2176
NEG = -30000.0
CAP = 256
NROW = 32 * CAP


class _Rec:
    def __init__(self):
        self.calls = []

    def __getattr__(self, name):
        def f(*a, **k):
            self.calls.append((name, a, k))
            return self
        return f


def _record(fn):
    r = _Rec()
    fn(r)
    assert len(r.calls) == 1, r.calls
    return r.calls[0]


class _Stop(Exception):
    pass


class Prog:
    def __init__(self, nc):
        self.nc = nc
        self.q = {e: [] for e in ENGS}
        self.cnt = {e: 0 for e in ENGS}
        self.res = {}
        self.known = {e: {} for e in ENGS}
        self.dcnt = [0] * NDSEM
        self.dnext = 0

    def _deps(self, eng, reads, writes):
        deps = {}

        def add(d):
            if d is None:
                return
            k, v = d
            if deps.get(k, 0) < v:
                deps[k] = v

        for r in reads:
            st = self.res.get(r)
            if st:
                add(st["w"])
        for w in writes:
            st = self.res.get(w)
            if st:
                add(st["w"])
                for d in st["r"]:
                    add(d)
        out = []
        for k, v in deps.items():
            if eng == PE and k == PE:
                continue
            if self.known[eng].get(k, 0) >= v:
                continue
            self.known[eng][k] = v
            out.append((k, v))
        return out

    def _mark(self, reads, writes, tok):
        for r in reads:
            st = self.res.setdefault(r, {"w": None, "r": []})
            st["r"].append(tok)
        for w in writes:
            self.res[w] = {"w": tok, "r": []}

    def op(self, eng, fn, reads=(), writes=()):
        deps = self._deps(eng, reads, writes)
        self.cnt[eng] += 1
        tok = (eng, self.cnt[eng])
        self.q[eng].append(("op", _record(fn), deps))
        self._mark(reads, writes, tok)
        return tok

    def dma(self, eng, fn, reads=(), writes=()):
        deps = self._deps(eng, reads, writes)
        s = self.dnext
        self.dnext = (self.dnext + 1) % NDSEM
        prev = self.dcnt[s]
        k = ("d", s)
        if prev and self.known[eng].get(k, 0) < prev:
            self.known[eng][k] = prev
            deps.append((k, prev))
        self.dcnt[s] += 16
        tok = (k, self.dcnt[s])
        self.q[eng].append(("dma", _record(fn), deps, s))
        self._mark(reads, writes, tok)
        return tok

    def barrier(self):
        for e in ENGS:
            deps = []
            for e2 in [PE, ACT, DVE, POOL]:
                v = self.cnt[e2]
                if v and self.known[e].get(e2, 0) < v and e2 != e:
                    self.known[e][e2] = v
                    deps.append((e2, v))
            for s in range(NDSEM):
                v = self.dcnt[s]
                k = ("d", s)
                if v and self.known[e].get(k, 0) < v:
                    self.known[e][k] = v
                    deps.append((k, v))
            self.q[e].append(("wait", None, deps))

    def emit(self):
        nc = self.nc
        if getattr(self, "flag", None) is not None:
            dflag, src = self.flag
            self.barrier()
            self.dma(SP, lambda e: e.dma_start(out=dflag, in_=src), writes=["dflag"])
            self.barrier()
        with contextlib.ExitStack() as st:
            sem = {}
            for e in [PE, ACT, DVE, POOL]:
                sem[e] = st.enter_context(nc.semaphore("s_" + e))
            for s in range(NDSEM):
                sem[("d", s)] = st.enter_context(nc.semaphore("s_d%d" % s))
            block = st.enter_context(nc.Block())

            def run(ename, eobj):
                bc_reg = None
                for item in self.q[ename]:
                    kind, fn, deps = item[0], item[1], item[2]
                    if kind == "dma" and fn[0] == "indirect_dma_start" and fn[2].get("bounds_check") is not None:
                        if bc_reg is None:
                            bc_reg = eobj.to_reg(fn[2]["bounds_check"])
                        kw = dict(fn[2]); kw["bounds_check"] = bc_reg
                        fn = (fn[0], fn[1], kw)
                    for k, v in deps:
                        eobj.wait_ge(sem[k], v)
                    if kind == "op":
                        getattr(eobj, fn[0])(*fn[1], **fn[2]).then_inc(sem[ename], 1)
                    elif kind == "dma":
                        getattr(eobj, fn[0])(*fn[1], **fn[2]).then_inc(sem[("d", item[3])], 16)

            @block.tensor
            def _(e):
                run(PE, e)

            @block.scalar
            def _(e):
                run(ACT, e)

            @block.vector
            def _(e):
                run(DVE, e)

            @block.gpsimd
            def _(e):
                run(POOL, e)

            @block.sync
            def _(e):
                run(SP, e)


def build_nc(stop=99, debug=False):
    nc = bass.Bass("TRN2", target_bir_lowering=False)

    def din(name, shape):
        return nc.dram_tensor(name, shape, F32, kind="ExternalInput").ap()

    def dscr(name, shape, ph=0):
        return nc.dram_tensor(name, shape, F32, kind="ExternalOutput" if (debug and stop == ph) else "Internal").ap()

    x_all = din("x_all", [NALL, 1024])
    w_in = din("w_in", [1024, WCOLS])
    gmix = din("gmix", [128, 8]); gmoe = din("gmoe", [128, 8]); gfin = din("gfin", [128, 1024])
    bgate = din("bgate", [128, 16]); sinks = din("sinks", [128, 8])
    m3_d = din("m3", [128, 1]); ident_d = din("ident", [128, 128]); jswap_d = din("jswap", [128, 128]); sgn_d = din("sgn", [128, 1])
    mcur_d = din("mcur", [128, 512]); mprev_d = din("mprev", [128, 512]); mprev0_d = din("mprev0", [128, 512])
    are1 = din("are1", [128, 32]); aim1 = din("aim1", [128, 32]); ldt1 = din("ldt1", [128, 32])
    are2 = din("are2", [128, 512]); aim2 = din("aim2", [128, 512]); ldt2 = din("ldt2", [128, 512])
    bre2 = din("bre2", [128, 512]); bim2 = din("bim2", [128, 512])
    cst_d = din("cst", [128, 512]); dpad_d = din("dpad", [128, 8])
    wglu = din("wglu", [1024, 1024]); bglu = din("bglu", [128, 8])
    wab = din("wab", [512, 1024]); wsb = din("wsb", [1024, 1024]); wout = din("wout", [1024, 1024])
    wr = din("wr", [128, 8, 36]); br = din("br", [128, 36])
    ustr_d = din("ustr", [128, 128]); ones_d = din("ones", [128, 128]); ecap_d = din("ecap", [128, 32])
    if stop > 3:
        weg = din("weg", [32, 1024, 512]); weu = din("weu", [32, 1024, 512]); wed = din("wed", [32, 512, 1024])
        y_out = nc.dram_tensor("y_out", [NOWN, 1024], F32, kind="ExternalOutput").ap()

    HT = dscr("HT", [8, 128, 2176], 1); UT = dscr("UT", [8, 128, NALL], 1); ZT = dscr("ZT", [8, 128, NOWN], 2)
    X1 = dscr("X1", [NOWN, 1024], 3.5)
    Xscr = dscr("Xscr", [NROW, 1024], 3.5); Yscr = dscr("Yscr", [NROW, 1024], 3.5)

    P = Prog(nc)
    pcnt = [0]
    dflag = nc.dram_tensor("dflag", [128, 128], F32, kind="ExternalOutput").ap() if debug else None

    with contextlib.ExitStack() as gst:
        def sb(st, name, shape, dt=F32):
            return st.enter_context(nc.sbuf_tensor("sb_" + name, shape, dt))

        banks = [gst.enter_context(nc.psum_tensor("bank%d" % i, [128, 512], F32)) for i in range(8)]

        def bank(i):
            return banks[i], ("ps", i)

        ident = sb(gst, "ident", [128, 128]); jswap = sb(gst, "jswap", [128, 128]); sgn = sb(gst, "sgn", [128, 1])
        identr = sb(gst, "identr", [128, 128], F32R); epsT = sb(gst, "epsT", [128, 1])
        P.op(DVE, lambda e: e.memset(epsT[:], EPS), writes=["epsT"])
        if debug:
            P.flag = (dflag, ident[:])
        P.dma(SP, lambda e: e.dma_start(out=ident[:], in_=ident_d), writes=["ident"])
        P.dma(SP, lambda e: e.dma_start(out=jswap[:], in_=jswap_d), writes=["jswap"])
        P.dma(SP, lambda e: e.dma_start(out=sgn[:], in_=sgn_d), writes=["sgn"])
        P.op(DVE, lambda e: e.tensor_copy(out=identr[:], in_=ident[:]), reads=["ident"], writes=["identr"])

        def norm_T(st_bufs, src_ap_fn, i, gfull, gkey, dst, dstkey, col0, tag):
            xt, xn, nxn, ss, rs = st_bufs
            s = i % 2
            sn = i % nxn
            xk = (tag + "xt", s); nk = (tag + "xn", sn)
            if src_ap_fn is not None:
                P.dma(SP, lambda e: e.dma_start(out=xt[:, s, :], in_=src_ap_fn(i)), writes=[xk])
            P.op(ACT, lambda e: e.activation(out=xn[:, sn, :], in_=xt[:, s, :], func=AF.Square, accum_out=ss[:, i:i + 1]),
                 reads=[xk], writes=[nk, (tag + "ss", i)])
            P.op(ACT, lambda e: e.activation(out=rs[:, i:i + 1], in_=ss[:, i:i + 1], func=AF.Sqrt, bias=epsT[:, 0:1], scale=1.0 / 1024),
                 reads=[(tag + "ss", i), "epsT"], writes=[(tag + "rs", i)])
            P.op(DVE, lambda e: e.reciprocal(out=rs[:, i:i + 1], in_=rs[:, i:i + 1]), reads=[(tag + "rs", i)], writes=[(tag + "rs", i)])
            P.op(DVE, lambda e: e.tensor_scalar(out=xn[:, sn, :], in0=xt[:, s, :], scalar1=rs[:, i:i + 1], scalar2=None, op0=ALU.mult),
                 reads=[xk, (tag + "rs", i), nk], writes=[nk])
            for hh in range(2):
                bi = pcnt[0] % 2; pcnt[0] += 1
                bk, bkey = bank(bi)
                for j in range(4):
                    k = hh * 4 + j
                    P.op(PE, lambda e, k=k, j=j, bk=bk: e.transpose(out=bk[:, j * 128:(j + 1) * 128], in_=xn[:, sn, k * 128:(k + 1) * 128], identity=ident[:]),
                         reads=[nk, "ident"], writes=[bkey])
                P.op(DVE, lambda e, hh=hh, bk=bk: e.tensor_tensor(out=dst[:, hh * 4:hh * 4 + 4, col0:col0 + 128],
                                                                 in0=bk[:].rearrange("p (k t) -> p k t", k=4),
                                                                 in1=gfull[:, hh * 4:hh * 4 + 4, :], op=ALU.mult),
                     reads=[bkey, gkey], writes=[dstkey])

        def make_gfull(st, name, g_d):
            g2 = sb(st, name + "2", [128, 8]); gf = sb(st, name + "f", [128, 8, 128])
            P.dma(SP, lambda e: e.dma_start(out=g2[:], in_=g_d), writes=[name + "2"])
            P.op(POOL, lambda e: e.memset(gf[:], 1.0), writes=[name])
            for k in range(8):
                P.op(POOL, lambda e, k=k: e.tensor_scalar(out=gf[:, k, :], in0=gf[:, k, :], scalar1=g2[:, k:k + 1], scalar2=None, op0=ALU.mult),
                     reads=[name + "2", name], writes=[name])
            return gf

        def wload(dst_ap, src_ap, key, reads=()):
            P.dma(POOL, lambda e: e.dma_start(out=dst_ap, in_=src_ap, max_dma_last_dim=8192), reads=list(reads), writes=[key])

        sst = contextlib.ExitStack()
        cs = sb(sst, "cs", [128, 13, 32]); dsg = sb(sst, "dsg", [128, 13, 32])
        Bst = sb(sst, "Bst", [128, 8, 128], F32R)
        Bst3 = sb(sst, "Bst3", [128, 8, 128], F32R); m3 = sb(sst, "m3", [128, 1])
        P.dma(SP, lambda e: e.dma_start(out=m3[:], in_=m3_d), writes=["m3"])
        Cpad = sb(sst, "Cpad", [128, 32, 128], F32R)
        Dpad = sb(sst, "Dpad", [128, 8])
        P.dma(SP, lambda e: e.dma_start(out=Dpad[:], in_=dpad_d), writes=["Dpad"])

        with contextlib.ExitStack() as st:
            def lam_bar(n, are_d, aim_d, ldt_d, tag):
                t = {nm: sb(st, tag + nm, [128, n]) for nm in ["are", "aim", "dt", "mag", "ang", "r", "c", "d", "x"]}
                t["qi"] = sb(st, tag + "qi", [128, n], mybir.dt.int32)
                P.dma(SP, lambda e: e.dma_start(out=t["are"][:], in_=are_d), writes=[tag + "are"])
                P.dma(SP, lambda e: e.dma_start(out=t["aim"][:], in_=aim_d), writes=[tag + "aim"])
                P.dma(SP, lambda e: e.dma_start(out=t["dt"][:], in_=ldt_d), writes=[tag + "dt"])
                K = lambda *a: [tag + x for x in a]
                P.op(ACT, lambda e: e.activation(out=t["dt"][:], in_=t["dt"][:], func=AF.Exp), reads=K("dt"), writes=K("dt"))
                P.op(DVE, lambda e: e.tensor_tensor(out=t["mag"][:], in0=t["are"][:], in1=t["dt"][:], op=ALU.mult), reads=K("are", "dt"), writes=K("mag"))
                P.op(ACT, lambda e: e.activation(out=t["mag"][:], in_=t["mag"][:], func=AF.Exp), reads=K("mag"), writes=K("mag"))
                P.op(DVE, lambda e: e.tensor_tensor(out=t["ang"][:], in0=t["aim"][:], in1=t["dt"][:], op=ALU.mult), reads=K("aim", "dt"), writes=K("ang"))
                for nm, off in (("d", 0.0), ("c", 0.5 * math.pi)):
                    P.op(DVE, lambda e, off=off: e.tensor_scalar(out=t["x"][:], in0=t["ang"][:], scalar1=off, scalar2=None, op0=ALU.add), reads=K("ang"), writes=K("x"))
                    P.op(DVE, lambda e: e.tensor_scalar(out=t["r"][:], in0=t["x"][:], scalar1=1.0 / (2 * math.pi), scalar2=None, op0=ALU.mult), reads=K("x"), writes=K("r"))
                    P.op(DVE, lambda e: e.tensor_copy(out=t["qi"][:], in_=t["r"][:]), reads=K("r"), writes=K("qi"))
                    P.op(DVE, lambda e: e.tensor_copy(out=t["r"][:], in_=t["qi"][:]), reads=K("qi"), writes=K("r"))
                    P.op(DVE, lambda e: e.scalar_tensor_tensor(out=t["r"][:], in0=t["r"][:], scalar=-2 * math.pi, in1=t["x"][:], op0=ALU.mult, op1=ALU.add), reads=K("r", "x"), writes=K("r"))
                    P.op(DVE, lambda e: e.tensor_scalar(out=t["x"][:], in0=t["r"][:], scalar1=math.pi, scalar2=2 * math.pi, op0=ALU.is_gt, op1=ALU.mult), reads=K("r"), writes=K("x"))
                    P.op(DVE, lambda e: e.tensor_tensor(out=t["r"][:], in0=t["r"][:], in1=t["x"][:], op=ALU.subtract), reads=K("r", "x"), writes=K("r"))
                    P.op(DVE, lambda e: e.tensor_scalar(out=t["r"][:], in0=t["r"][:], scalar1=math.pi, scalar2=-math.pi, op0=ALU.min, op1=ALU.max), reads=K("r"), writes=K("r"))
                    P.op(ACT, lambda e, nm=nm: e.activation(out=t[nm][:], in_=t["r"][:], func=AF.Sin), reads=K("r"), writes=K(nm))
                    P.op(DVE, lambda e, nm=nm: e.tensor_tensor(out=t[nm][:], in0=t[nm][:], in1=t["mag"][:], op=ALU.mult), reads=K(nm, "mag"), writes=K(nm))
                return t

            t1 = lam_bar(32, are1, aim1, ldt1, "l1")
            tmpa = sb(st, "tmpa", [128, 32]); tmpb = sb(st, "tmpb", [128, 32]); dd = sb(st, "dd", [128, 13, 32])
            P.op(DVE, lambda e: e.tensor_copy(out=cs[:, 0, :], in_=t1["c"][:]), reads=["l1c"], writes=["cs"])
            P.op(DVE, lambda e: e.tensor_copy(out=dd[:, 0, :], in_=t1["d"][:]), reads=["l1d"], writes=["dd"])
            for k in range(1, 13):
                P.op(DVE, lambda e, k=k: e.tensor_tensor(out=tmpa[:], in0=cs[:, k - 1, :], in1=cs[:, k - 1, :], op=ALU.mult), reads=["cs"], writes=["tmpa"])
                P.op(DVE, lambda e, k=k: e.tensor_tensor(out=tmpb[:], in0=dd[:, k - 1, :], in1=dd[:, k - 1, :], op=ALU.mult), reads=["dd"], writes=["tmpb"])
                P.op(DVE, lambda e, k=k: e.scalar_tensor_tensor(out=dd[:, k, :], in0=cs[:, k - 1, :], scalar=2.0, in1=dd[:, k - 1, :], op0=ALU.mult, op1=ALU.mult),
                     reads=["cs", "dd"], writes=["dd"])
                P.op(DVE, lambda e, k=k: e.tensor_tensor(out=cs[:, k, :], in0=tmpa[:], in1=tmpb[:], op=ALU.subtract), reads=["tmpa", "tmpb", "cs"], writes=["cs"])
            P.op(DVE, lambda e: e.tensor_scalar(out=dsg[:], in0=dd[:], scalar1=sgn[:, 0:1], scalar2=None, op0=ALU.mult), reads=["dd", "sgn"], writes=["dsg"])

            t2 = lam_bar(512, are2, aim2, ldt2, "l2")
            br_t = sb(st, "br_t", [128, 512]); bi_t = sb(st, "bi_t", [128, 512])
            P.dma(SP, lambda e: e.dma_start(out=br_t[:], in_=bre2), writes=["br_t"])
            P.dma(SP, lambda e: e.dma_start(out=bi_t[:], in_=bim2), writes=["bi_t"])
            den = sb(st, "den", [128, 512]); u1 = sb(st, "u1", [128, 512]); u2 = sb(st, "u2", [128, 512])
            kr = sb(st, "kr", [128, 512]); ki = sb(st, "ki", [128, 512])
            TT = lambda o, a, b, op, rd, wr_: P.op(DVE, lambda e: e.tensor_tensor(out=o, in0=a, in1=b, op=op), reads=rd, writes=wr_)
            TT(den[:], t2["are"][:], t2["are"][:], ALU.mult, ["l2are"], ["den"])
            TT(u1[:], t2["aim"][:], t2["aim"][:], ALU.mult, ["l2aim"], ["u1"])
            TT(den[:], den[:], u1[:], ALU.add, ["den", "u1"], ["den"])
            P.op(DVE, lambda e: e.reciprocal(out=den[:], in_=den[:]), reads=["den"], writes=["den"])
            P.op(DVE, lambda e: e.tensor_scalar(out=t2["c"][:], in0=t2["c"][:], scalar1=-1.0, scalar2=None, op0=ALU.add), reads=["l2c"], writes=["l2c"])
            TT(u1[:], t2["c"][:], t2["are"][:], ALU.mult, ["l2c", "l2are"], ["u1"])
            TT(u2[:], t2["d"][:], t2["aim"][:], ALU.mult, ["l2d", "l2aim"], ["u2"])
            TT(kr[:], u1[:], u2[:], ALU.add, ["u1", "u2"], ["kr"])
            TT(kr[:], kr[:], den[:], ALU.mult, ["kr", "den"], ["kr"])
            TT(u1[:], t2["d"][:], t2["are"][:], ALU.mult, ["l2d", "l2are"], ["u1"])
            TT(u2[:], t2["c"][:], t2["aim"][:], ALU.mult, ["l2c", "l2aim"], ["u2"])
            TT(ki[:], u1[:], u2[:], ALU.subtract, ["u1", "u2"], ["ki"])
            TT(ki[:], ki[:], den[:], ALU.mult, ["ki", "den"], ["ki"])
            v3 = lambda a: a[:].rearrange("p (q n) -> p q n", q=8)
            TT(u1[:], kr[:], br_t[:], ALU.mult, ["kr", "br_t"], ["u1"])
            TT(u2[:], ki[:], bi_t[:], ALU.mult, ["ki", "bi_t"], ["u2"])
            TT(Bst[:, :, 0:64], v3(u1), v3(u2), ALU.subtract, ["u1", "u2"], ["Bst"])
            TT(u1[:], kr[:], bi_t[:], ALU.mult, ["kr", "bi_t"], ["u1"])
            TT(u2[:], ki[:], br_t[:], ALU.mult, ["ki", "br_t"], ["u2"])
            TT(Bst[:, :, 64:128], v3(u1), v3(u2), ALU.add, ["u1", "u2", "Bst"], ["Bst"])
            P.op(DVE, lambda e: e.tensor_scalar(out=Bst3[:], in0=Bst[:].bitcast(F32), scalar1=m3[:, 0:1], scalar2=None, op0=ALU.mult), reads=["Bst", "m3"], writes=["Bst3"])
            cst = sb(st, "cst", [128, 32, 16])
            P.dma(SP, lambda e: e.dma_start(out=cst[:].rearrange("p g c -> p (g c)"), in_=cst_d), writes=["cst"])
            for g_ in range(32):
                P.op(POOL, lambda e, g_=g_: e.tensor_scalar(out=Cpad[:, g_, :], in0=ident[:], scalar1=0.0, scalar2=None, op0=ALU.mult), reads=["ident", "Cpad"], writes=["Cpad"])
            for gi in range(4):
                P.op(DVE, lambda e, gi=gi: e.tensor_copy(out=Cpad[0:64, gi::4, 32 * gi:32 * gi + 16], in_=cst[0:64, gi::4, :]), reads=["cst", "Cpad"], writes=["Cpad"])
                P.op(DVE, lambda e, gi=gi: e.tensor_scalar(out=Cpad[64:128, gi::4, 32 * gi:32 * gi + 16], in0=cst[64:128, gi::4, :], scalar1=-1.0, scalar2=None, op0=ALU.mult),
                     reads=["cst", "Cpad"], writes=["Cpad"])

            zt = sb(st, "zt", [128, 2048])
            P.op(DVE, lambda e: e.memset(zt[:], 0.0), writes=["zt"])
            xz = Xscr.rearrange("(p r) c -> p r c", p=128)
            for r_ in range(0, 64, 2):
                P.dma(SP, lambda e, r_=r_: e.dma_start(out=xz[:, r_:r_ + 2, :], in_=zt[:].rearrange("p (r c) -> p r c", r=2)), reads=["zt"], writes=[("Xz", r_)])
            gmixf = make_gfull(st, "gmix", gmix)
            xt = sb(st, "a_xt", [128, 2, 1024]); xn = sb(st, "a_xn", [128, 2, 1024]); junk = 2
            ss = sb(st, "a_ss", [128, NT]); rs = sb(st, "a_rs", [128, NT])
            bufs = (xt, xn, junk, ss, rs)
            wU = sb(st, "wU", [128, 8, 1024], F32R)
            wload(wU[:], w_in.rearrange("(k p) c -> p k c", p=128)[:, :, CU:CU + 1024], "wU")
            hTg = sb(st, "hTg", [128, 2, 8, 512], F32R); UTg = sb(st, "UTg", [128, 2, 8, 512])
            for grp in range(16):
                gs = grp % 2
                hk = ("hTg", gs)
                for j in range(4):
                    i = grp * 4 + j
                    norm_T(bufs, lambda i: x_all[i * 128:(i + 1) * 128, :], i, gmixf, "gmix", hTg[:, gs], hk, j * 128, "a_")
                for ch in range(8):
                    bi = 2 + (ch % 4)
                    bk, bkey = bank(bi)
                    for k in range(8):
                        P.op(PE, lambda e, k=k, ch=ch, bk=bk: e.matmul(bk[:], wU[:, k, ch * 128:(ch + 1) * 128], hTg[:, gs, k, :], start=(k == 0), stop=(k == 7)),
                             reads=["wU", hk], writes=[bkey])
                    P.op(ACT, lambda e, ch=ch, bk=bk: e.copy(out=UTg[:, gs, ch, :], in_=bk[:]), reads=[bkey], writes=[("UTg", gs)])
                P.dma(ACT, lambda e, grp=grp: e.dma_start(out=UT[:, :, grp * 512:(grp + 1) * 512].rearrange("k p t -> p k t"), in_=UTg[:, gs]),
                      reads=[("UTg", gs)], writes=[("UT", grp)])
                if grp == 11:
                    P.dma(ACT, lambda e: e.dma_start(out=HT[:, :, 0:128].rearrange("k p t -> p k t"), in_=hTg[:, gs, :, 384:512].bitcast(F32)),
                          reads=[hk], writes=[("HT", 0)])
                if grp >= 12:
                    c0 = 128 + (grp - 12) * 512
                    P.dma(ACT, lambda e, c0=c0: e.dma_start(out=HT[:, :, c0:c0 + 512].rearrange("k p t -> p k t"), in_=hTg[:, gs].bitcast(F32)),
                          reads=[hk], writes=[("HT", grp - 11)])
        P.barrier()
        if stop <= 1:
            sst.close(); P.emit(); globals()["LASTP"] = P; return nc

        with contextlib.ExitStack() as st:
            UTc = sb(st, "UTc", [128, NALL], F32R)
            Xs = [sb(st, "X%d" % i, [128, NALL], F32R) for i in range(4)]
            ATl = sb(st, "ATl", [128, 2, 4, 128], F32R); lvl = [0]
            tmpJ4 = sb(st, "tmpJ4", [128, 4, 128])
            yv = sb(st, "yv", [128, 2, 512]); ta = sb(st, "ta", [128, 2, 512]); zs = ta
            ycnt = 0
            for q in range(8):
                wload(UTc[:], UT[q], "UTc", reads=[("UT", g_) for g_ in range(16)])
                for gi in range(4):
                    X = Xs[gi]
                    for b in range(16):
                        bi = b % 4
                        bk, bkey = bank(bi)
                        P.op(PE, lambda e, b=b, bk=bk, X=X, gi=gi: e.matmul(bk[:], (Bst[32 * gi:32 * gi + 32, q, :] if gi < 3 else Bst3[64:128, q, :]), (UTc[32 * gi:32 * gi + 32, b * 512:(b + 1) * 512] if gi < 3 else UTc[64:128, b * 512:(b + 1) * 512]), start=True, stop=True),
                             reads=["Bst", "Bst3", "UTc"], writes=[bkey])
                        if b % 2 == 0:
                            P.op(ACT, lambda e, b=b, bk=bk, X=X: e.copy(out=X[:, b * 512:(b + 1) * 512], in_=bk[:]), reads=[bkey], writes=[("X", gi, b)])
                        else:
                            P.op(DVE, lambda e, b=b, bk=bk, X=X: e.tensor_copy(out=X[:, b * 512:(b + 1) * 512], in_=bk[:]), reads=[bkey], writes=[("X", gi, b)])
                def flush(pend, stride):
                    for gi_, X_, bk_, bkey_, t0, na in pend:
                        P.op(DVE, lambda e, bk_=bk_, X_=X_, t0=t0, na=na, stride=stride: e.tensor_tensor(out=X_[:, t0:t0 + (na - 1) * stride + 1:stride], in0=bk_[:, 0:na], in1=X_[:, t0:t0 + (na - 1) * stride + 1:stride].bitcast(F32), op=ALU.add),
                             reads=[bkey_, ("Xg", gi_)], writes=[("Xg", gi_)])
                    del pend[:]

                levels = [(1 << k, True) for k in range(13)] + [(1 << k, False) for k in range(11, -1, -1)]
                for li, (d, up) in enumerate(levels):
                    k = d.bit_length() - 1
                    slot = lvl[0] % 2; lvl[0] += 1
                    for gi in range(4):
                        g = 4 * q + gi
                        P.op(ACT, lambda e, k=k, g=g, gi=gi: e.activation(out=tmpJ4[:, gi, :], in_=jswap[:], func=AF.Copy, scale=dsg[:, k, g:g + 1]),
                             reads=["jswap", "dsg"], writes=[("tmpJ4", gi)])
                        P.op(DVE, lambda e, k=k, g=g, slot=slot, gi=gi: e.scalar_tensor_tensor(out=ATl[:, slot, gi, :], in0=ident[:], scalar=cs[:, k, g:g + 1], in1=tmpJ4[:, gi, :], op0=ALU.mult, op1=ALU.add),
                             reads=["ident", "cs", ("tmpJ4", gi)], writes=[("ATl", slot, gi)])
                    if up:
                        src0, dst0, stride, cnt = d - 1, 2 * d - 1, 2 * d, NALL // (2 * d)
                    elif d == 2048:
                        src0, dst0, stride, cnt = 4095, 6143, 4096, 1
                    else:
                        src0, dst0, stride, cnt = 6143, 6143 + d, 2 * d, 1024 // d
                    if cnt == 1:
                        stride = NALL - 1 - src0
                    cnt_mm = cnt + (cnt % 2)
                    pend = []
                    for gi in range(4):
                        X = Xs[gi]
                        for p0 in range(0, cnt_mm, 512):
                            n = min(512, cnt_mm - p0)
                            na = min(n, cnt - p0)
                            bi = pcnt[0] % 8; pcnt[0] += 1
                            bk, bkey = bank(bi)
                            a0 = src0 + p0 * stride
                            rd = [("Xg", gi), ("ATl", slot, gi)] + ([("X", gi, b_) for b_ in range(16)] if li == 0 else [])
                            P.op(PE, lambda e, bk=bk, X=X, a0=a0, n=n, stride=stride, slot=slot, gi=gi: e.matmul(bk[:, 0:n], ATl[:, slot, gi, :], X[:, a0:a0 + (n - 1) * stride + 1:stride], start=True, stop=True),
                                 reads=rd, writes=[bkey])
                            pend.append((gi, X, bk, bkey, dst0 + p0 * stride, na))
                            if len(pend) == 8:
                                flush(pend, stride)
                    flush(pend, stride)
                for tb in range(4):
                    c0 = 6144 + tb * 512
                    bi = tb % 4
                    bk, bkey = bank(bi)
                    for gi in range(4):
                        P.op(PE, lambda e, gi=gi, bk=bk, c0=c0: e.matmul(bk[:], Cpad[:, 4 * q + gi, :], Xs[gi][:, c0:c0 + 512], start=(gi == 0), stop=(gi == 3)),
                             reads=["Cpad", ("Xg", gi)] + [("X", gi, b_) for b_ in range(16)], writes=[bkey])
                    ys = ycnt % 2; ycnt += 1
                    yk, tk, zk = ("yv", ys), ("ta", ys), ("zs", ys)
                    P.op(DVE, lambda e, bk=bk, c0=c0, ys=ys: e.scalar_tensor_tensor(out=yv[:, ys, :], in0=UTc[:, c0:c0 + 512].bitcast(F32), scalar=Dpad[:, q:q + 1], in1=bk[:], op0=ALU.mult, op1=ALU.add),
                         reads=["UTc", "Dpad", bkey], writes=[yk])
                    P.op(POOL, lambda e, ys=ys: e.tensor_tensor(out=ta[:, ys, :], in0=yv[:, ys, :], in1=yv[:, ys, :], op=ALU.mult), reads=[yk], writes=[tk])
                    P.op(POOL, lambda e, ys=ys: e.tensor_scalar(out=ta[:, ys, :], in0=ta[:, ys, :], scalar1=0.044715, scalar2=1.0, op0=ALU.mult, op1=ALU.add), reads=[tk], writes=[tk])
                    P.op(POOL, lambda e, ys=ys: e.tensor_tensor(out=ta[:, ys, :], in0=ta[:, ys, :], in1=yv[:, ys, :], op=ALU.mult), reads=[tk, yk], writes=[tk])
                    P.op(ACT, lambda e, ys=ys: e.activation(out=ta[:, ys, :], in_=ta[:, ys, :], func=AF.Sigmoid, scale=1.5957691216), reads=[tk], writes=[tk])
                    P.op(POOL, lambda e, ys=ys: e.tensor_tensor(out=zs[:, ys, :], in0=ta[:, ys, :], in1=yv[:, ys, :], op=ALU.mult), reads=[tk, yk], writes=[tk])
                    P.dma(ACT, lambda e, ys=ys, tb=tb: e.dma_start(out=ZT[q, :, tb * 512:(tb + 1) * 512], in_=zs[:, ys, :]), reads=[tk], writes=[("ZT", q, tb)])
        P.barrier()
        sst.close()
        if stop <= 2:
            P.emit(); return nc

        try:
            wdense_cm = contextlib.ExitStack()
            wts = sb(wdense_cm, "wts", [128, 16, 2]); idxi = sb(wdense_cm, "idxi", [128, 32], mybir.dt.int32)
            with contextlib.ExitStack() as st:
                KT = sb(st, "KT", [128, 4, 640], F32R)
                VA = sb(st, "VA", [128, 17, 132], F32R)
                mcur = sb(st, "mcur", [128, 512], F32R); mprev = sb(st, "mprev", [128, 512], F32R); mprev0 = sb(st, "mprev0", [128, 512], F32R)
                wload(mcur[:], mcur_d, "mcur"); wload(mprev[:], mprev_d, "mprev"); wload(mprev0[:], mprev0_d, "mprev0")
                sinke = sb(st, "sinke", [128, 8]); bg = sb(st, "bg", [128, 16]); bgl = sb(st, "bgl", [128, 8])
                P.dma(SP, lambda e: e.dma_start(out=sinke[:], in_=sinks), writes=["sinke"])
                P.op(ACT, lambda e: e.activation(out=sinke[:], in_=sinke[:], func=AF.Exp), reads=["sinke"], writes=["sinke"])
                P.dma(SP, lambda e: e.dma_start(out=bg[:], in_=bgate), writes=["bg"])
                P.dma(SP, lambda e: e.dma_start(out=bgl[:], in_=bglu), writes=["bgl"])
                i17 = ident[:, 0:17].rearrange("p (a b) -> p a b", b=1)
                P.op(POOL, lambda e: e.tensor_scalar(out=VA[:, :, 64:65], in0=i17, scalar1=0.0, scalar2=1.0, op0=ALU.mult, op1=ALU.add), reads=["ident"], writes=["VAones"])
                P.op(POOL, lambda e: e.tensor_scalar(out=VA[:, :, 130:131], in0=i17, scalar1=0.0, scalar2=1.0, op0=ALU.mult, op1=ALU.add), reads=["ident"], writes=["VAones2"])
                P.op(POOL, lambda e: e.tensor_scalar(out=VA[:, :, 65:66], in0=i17, scalar1=0.0, scalar2=None, op0=ALU.mult), reads=["ident", "VAones"], writes=["VAones"])
                P.op(POOL, lambda e: e.tensor_scalar(out=VA[:, :, 131:132], in0=i17, scalar1=0.0, scalar2=None, op0=ALU.mult), reads=["ident", "VAones2"], writes=["VAones2"])
                gmoef = make_gfull(st, "gmoe", gmoe)
                wrt = sb(st, "wrt", [128, 8, 36]); brt = sb(st, "brt", [128, 36])
                P.dma(SP, lambda e: e.dma_start(out=wrt[:], in_=wr), writes=["wrt"])
                P.dma(SP, lambda e: e.dma_start(out=brt[:], in_=br), writes=["brt"])
                ring = [sb(st, "ring%d" % i, [128, 4096], F32R) for i in range(4)]
                rc = [0]

                def wpanel(src_ap, shape_str, k):
                    i = rc[0] % 4; rc[0] += 1
                    c = src_ap.shape[-1]
                    v = ring[i][:, 0:k * c].rearrange(shape_str, k=k)
                    wload(v, src_ap, ("ring", i))
                    return v, ("ring", i)

                w_in_v = w_in.rearrange("(k p) c -> p k c", p=128)
                hT = sb(st, "hT", [128, 8, 512], F32R)
                qp = sb(st, "qp", [128, 8, 512], F32R)
                QT = qp[:, 0:4, :]
                PT = qp[:, 4:8, :].rearrange("p (a b) t -> p a b t", a=2)
                atok = sb(st, "atok", [128, 512]); rden = sb(st, "rden", [128, 8])
                aT = sb(st, "aT", [128, 4, 512], F32R)
                zT = sb(st, "zT", [128, 8, 512], F32R); z2T = sb(st, "z2T", [128, 8, 512], F32R)
                sg = sb(st, "sg", [128, 2, 512]); mt = sb(st, "mt", [128, 2, 512])
                mgT = qp
                xt = sb(st, "b_xt", [128, 2, 1024]); xn = sb(st, "b_xn", [128, 2, 1024]); junk = 2
                ustr = sb(st, "ustr", [128, 128]); ones_t = sb(st, "ones_t", [128, 128]); ecap = sb(st, "ecap", [128, 32]); cntb = sb(st, "cntb", [128, 32])
                Am = sb(st, "Am", [128, 32]); posf = sb(st, "posf", [128, 32]); ptmp = sb(st, "ptmp", [128, 32]); idxf = sb(st, "idxf", [128, 4])
                P.dma(SP, lambda e: e.dma_start(out=ustr[:], in_=ustr_d), writes=["ustr"])
                P.dma(SP, lambda e: e.dma_start(out=ones_t[:], in_=ones_d), writes=["ones_t"])
                P.dma(SP, lambda e: e.dma_start(out=ecap[:], in_=ecap_d), writes=["ecap"])
                P.op(DVE, lambda e: e.memset(cntb[:], 0.0), writes=["cntb"])
                ss = sb(st, "b_ss", [128, 16]); rs = sb(st, "b_rs", [128, 16])
                h2g = hT
                lg = sb(st, "lg", [128, 36]); rt = {nm: sb(st, "rt_" + nm, [128, 40]) for nm in ["a", "b", "c", "d", "e", "f", "g", "h"]}
                wload(hT[:, :, 0:128], HT[:, :, 0:128].rearrange("k p t -> p k t"), "hT", reads=[("HT", 0)])

                def proj_fm(wv, wkey, ncol_chunks, rhs_fn, rkey, N, out_fn, okeys_fn, evac_scale=None, nk=8):
                    for ch in range(ncol_chunks):
                        bi = pcnt[0] % 4; pcnt[0] += 1
                        bk, bkey = bank(bi)
                        for k in range(nk):
                            P.op(PE, lambda e, k=k, ch=ch, bk=bk: e.matmul(bk[:, 0:N], wv[:, k, ch * 128:(ch + 1) * 128], rhs_fn(k), start=(k == 0), stop=(k == nk - 1)),
                                 reads=[wkey, rkey], writes=[bkey])
                        if evac_scale is None:
                            P.op(ACT, lambda e, ch=ch, bk=bk: e.copy(out=out_fn(ch), in_=bk[:, 0:N]), reads=[bkey], writes=okeys_fn(ch))
                        else:
                            P.op(ACT, lambda e, ch=ch, bk=bk: e.mul(out=out_fn(ch), in_=bk[:, 0:N], mul=evac_scale), reads=[bkey], writes=okeys_fn(ch))

                def kv_proj(hsrc, hkey, N, col0, tiles):
                    wv, wk = wpanel(w_in_v[:, :, CK:CK + 512], "p (k c) -> p k c", k=8)
                    proj_fm(wv, wk, 4, lambda k: hsrc[:, k, 0:N], hkey, N, lambda ch: KT[:, ch, col0:col0 + N], lambda ch: [("KT", ch)])
                    wv, wk = wpanel(w_in_v[:, :, CV:CV + 128], "p (k c) -> p k c", k=8)
                    for ti, tile_idx in enumerate(tiles):
                        bi = pcnt[0] % 4; pcnt[0] += 1
                        bk, bkey = bank(bi)
                        for k in range(8):
                            P.op(PE, lambda e, k=k, ti=ti, bk=bk: e.matmul(bk[:, 0:128], hsrc[:, k, ti * 128:(ti + 1) * 128], wv[:, k, :], start=(k == 0), stop=(k == 7)),
                                 reads=[wk, hkey], writes=[bkey])
                        P.op(ACT, lambda e, bk=bk, tile_idx=tile_idx: e.copy(out=VA[:, tile_idx, 0:64], in_=bk[:, 0:64]), reads=[bkey], writes=[("VA", tile_idx, 0)])
                        P.op(ACT, lambda e, bk=bk, tile_idx=tile_idx: e.copy(out=VA[:, tile_idx, 66:130], in_=bk[:, 64:128]), reads=[bkey], writes=[("VA", tile_idx, 1)])

                kv_proj(hT, "hT", 128, 0, [0])
                if stop <= 2.2:
                    st.close()
                    raise _Stop

                for tg in range(4):
                    wload(hT[:], HT[:, :, 128 + tg * 512:128 + (tg + 1) * 512].rearrange("k p t -> p k t"), "hT", reads=[("HT", tg + 1)])
                    wload(zT[:], ZT[:, :, tg * 512:(tg + 1) * 512].rearrange("k p t -> p k t"), "zT", reads=[("ZT", q_, tg) for q_ in range(8)])
                    wv, wk = wpanel(w_in_v[:, :, CQ:CQ + 512], "p (k c) -> p k c", k=8)
                    proj_fm(wv, wk, 4, lambda k: hT[:, k, :], "hT", 512, lambda ch: QT[:, ch, :], lambda ch: [("qp", ch)], evac_scale=0.125)
                    if tg > 0:
                        P.op(DVE, lambda e: e.tensor_copy(out=KT[:, :, 0:128], in_=KT[:, :, 512:640]), reads=[("KT", c_) for c_ in range(4)], writes=[("KT", c_) for c_ in range(4)])
                    kv_proj(hT, "hT", 512, 128, [1 + tg * 4 + j for j in range(4)])
                    if stop <= 2.4 and tg == DBG_TG:
                        st.close()
                        raise _Stop
                    for qb in range(4):
                        blk = 1 + tg * 4 + qb
                        obanks = []
                        for kv in range(2):
                            for kbi, kb in enumerate((blk - 1, blk)):
                                bi = 4 + (pcnt[0] % 2); pcnt[0] += 1
                                bk, bkey = bank(bi)
                                if kbi == 1:
                                    mk, mkey = mcur, "mcur"
                                elif blk == 1:
                                    mk, mkey = mprev0, "mprev0"
                                else:
                                    mk, mkey = mprev, "mprev"
                                P.op(PE, lambda e, bk=bk, mk=mk: e.matmul(bk[:], identr[:], mk[:], start=True, stop=False), reads=["identr", mkey], writes=[bkey])
                                for hh in range(4):
                                    h = kv * 4 + hh
                                    half = h % 2
                                    P.op(PE, lambda e, bk=bk, hh=hh, h=h, half=half, kb=kb, kv=kv, qb=qb, kbi=kbi: e.matmul(
                                        bk[:, hh * 128:(hh + 1) * 128], KT[:, kv * 2 + half, (qb + kbi) * 128:(qb + kbi + 1) * 128],
                                        QT[:, h // 2, qb * 128:(qb + 1) * 128], start=False, stop=(hh == 3)),
                                        reads=[("KT", kv * 2 + half), ("qp", h // 2)], writes=[bkey])
                                P.op(ACT, lambda e, bk=bk, kv=kv, kbi=kbi: e.activation(out=PT[:, kv, kbi, :], in_=bk[:], func=AF.Exp), reads=[bkey], writes=[("qp", 4 + kv * 2 + kbi)])
                            if stop <= 2.45 and tg == DBG_TG:
                                st.close()
                                raise _Stop
                            ob = 6 + kv
                            bko, bokey = bank(ob)
                            for hh in range(4):
                                for kbi, kb in enumerate((blk - 1, blk)):
                                    P.op(PE, lambda e, bko=bko, hh=hh, kbi=kbi, kb=kb, kv=kv: e.matmul(
                                        bko[:, hh * 66:(hh + 1) * 66], PT[:, kv, kbi, hh * 128:(hh + 1) * 128], VA[:, kb, kv * 66:(kv + 1) * 66],
                                        start=(kbi == 0), stop=(kbi == 1)),
                                        reads=[("qp", 4 + kv * 2 + kbi), ("VA", kb, kv), "VAones", "VAones2"], writes=[bokey])
                            if stop <= 2.5 and tg == DBG_TG:
                                st.close()
                                raise _Stop
                            ov = bko[:, 0:264].rearrange("p (h d) -> p h d", h=4)
                            P.op(DVE, lambda e, ov=ov, kv=kv: e.tensor_tensor(out=rden[:, kv * 4:(kv + 1) * 4], in0=ov[:, :, 64], in1=sinke[:, kv * 4:(kv + 1) * 4], op=ALU.add),
                                 reads=[bokey, "sinke"], writes=[("rden", kv)])
                            P.op(DVE, lambda e, kv=kv: e.reciprocal(out=rden[:, kv * 4:(kv + 1) * 4], in_=rden[:, kv * 4:(kv + 1) * 4]), reads=[("rden", kv)], writes=[("rden", kv)])
                            for hh in range(4):
                                h = kv * 4 + hh
                                P.op(DVE, lambda e, ov=ov, hh=hh, h=h: e.tensor_scalar(out=atok[:, h * 64:(h + 1) * 64], in0=ov[:, hh, 0:64], scalar1=rden[:, h:h + 1], scalar2=None, op0=ALU.mult),
                                     reads=[bokey, ("rden", kv)], writes=[("atok", kv)])
                        if stop <= 2.55 and tg == DBG_TG:
                            st.close()
                            raise _Stop
                        bi = pcnt[0] % 4; pcnt[0] += 1
                        bk, bkey = bank(bi)
                        for j in range(4):
                            P.op(PE, lambda e, j=j, bk=bk: e.transpose(out=bk[:, j * 128:(j + 1) * 128], in_=atok[:, j * 128:(j + 1) * 128], identity=ident[:]),
                                 reads=[("atok", 0), ("atok", 1), "ident"], writes=[bkey])
                        P.op(ACT, lambda e, bk=bk, qb=qb: e.copy(out=aT[:, :, qb * 128:(qb + 1) * 128], in_=bk[:].rearrange("p (k t) -> p k t", k=4)), reads=[bkey], writes=[("aT", qb)])
                    aTk = [("aT", qb) for qb in range(4)]
                    if stop <= 2.6 and tg == DBG_TG:
                        st.close()
                        raise _Stop
                    for hf in range(2):
                        wv, wk = wpanel(wglu.rearrange("(k p) c -> p k c", p=128)[:, :, hf * 512:(hf + 1) * 512], "p (k c) -> p k c", k=8)
                        for c4 in range(4):
                            ch = hf * 4 + c4
                            bi = pcnt[0] % 4; pcnt[0] += 1
                            bk, bkey = bank(bi)
                            for k in range(8):
                                P.op(PE, lambda e, k=k, c4=c4, bk=bk: e.matmul(bk[:], wv[:, k, c4 * 128:(c4 + 1) * 128], zT[:, k, :], start=(k == 0), stop=(k == 7)), reads=[wk, "zT"], writes=[bkey])
                            s_ = ch % 2
                            P.op(ACT, lambda e, ch=ch, bk=bk, s_=s_: e.activation(out=sg[:, s_, :], in_=bk[:], func=AF.Sigmoid, bias=bgl[:, ch:ch + 1]), reads=[bkey, "bgl"], writes=[("sg", s_)])
                            P.op(POOL, lambda e, ch=ch, s_=s_: e.tensor_tensor(out=z2T[:, ch, :], in0=sg[:, s_, :], in1=zT[:, ch, :].bitcast(F32), op=ALU.mult), reads=[("sg", s_), "zT"], writes=[("z2T", ch)])
                    z2k = [("z2T", ch) for ch in range(8)]
                    for hf in range(2):
                        wab_v, wab_k = wpanel(wab.rearrange("(k p) c -> p k c", p=128)[:, :, hf * 512:(hf + 1) * 512], "p (k c) -> p k c", k=4)
                        wsb_v, wsb_k = wpanel(wsb.rearrange("(k p) c -> p k c", p=128)[:, :, hf * 512:(hf + 1) * 512], "p (k c) -> p k c", k=8)
                        wga_v, wga_k = wpanel(w_in_v[:, :, CG + hf * 512:CG + (hf + 1) * 512], "p (k c) -> p k c", k=8)
                        wgs_v, wgs_k = wpanel(w_in_v[:, :, CG + 1024 + hf * 512:CG + 1024 + (hf + 1) * 512], "p (k c) -> p k c", k=8)
                        for c4 in range(4):
                            dmc = hf * 4 + c4
                            specs = [(wga_v, wga_k, 8, lambda k: hT[:, k, :], ["hT"]), (wab_v, wab_k, 4, lambda k: aT[:, k, :], aTk),
                                     (wgs_v, wgs_k, 8, lambda k: hT[:, k, :], ["hT"]), (wsb_v, wsb_k, 8, lambda k: z2T[:, k, :], z2k)]
                            bks = []
                            for si, (wv_, wk_, nk, rf, rk) in enumerate(specs):
                                bi = si
                                bk, bkey = bank(bi)
                                bks.append((bk, bkey))
                                for k in range(nk):
                                    P.op(PE, lambda e, k=k, c4=c4, bk=bk, wv_=wv_, rf=rf, nk=nk: e.matmul(bk[:], wv_[:, k, c4 * 128:(c4 + 1) * 128], rf(k), start=(k == 0), stop=(k == nk - 1)),
                                         reads=[wk_] + rk, writes=[bkey])
                            for br_i in range(2):
                                (gk, gkey), (vk, vkey) = bks[2 * br_i], bks[2 * br_i + 1]
                                bcol = br_i * 8 + dmc
                                P.op(ACT, lambda e, gk=gk, bcol=bcol, br_i=br_i: e.activation(out=sg[:, br_i, :], in_=gk[:], func=AF.Sigmoid, bias=bg[:, bcol:bcol + 1]), reads=[gkey, "bg"], writes=[("sg", br_i)])
                                P.op(DVE, lambda e, vk=vk, br_i=br_i: e.tensor_tensor(out=mt[:, br_i, :], in0=vk[:], in1=sg[:, br_i, :], op=ALU.mult), reads=[vkey, ("sg", br_i)], writes=[("mt", br_i)])
                            P.op(POOL, lambda e, dmc=dmc: e.tensor_tensor(out=mgT[:, dmc, :], in0=mt[:, 0, :], in1=mt[:, 1, :], op=ALU.add), reads=[("mt", 0), ("mt", 1)], writes=[("qp", dmc)])
                    mgk = [("qp", c_) for c_ in range(8)]
                    if stop <= 2.8 and tg == DBG_TG:
                        st.close()
                        raise _Stop
                    wo = []
                    for hf in range(2):
                        wo.append(wpanel(wout.rearrange("(k p) c -> p k c", p=128)[:, :, hf * 512:(hf + 1) * 512], "p (k c) -> p k c", k=8))
                    for j in range(4):
                        ti = tg * 4 + j
                        s = ti % 2
                        xk = ("b_xt", s)
                        P.dma(SP, lambda e, ti=ti, s=s: e.dma_start(out=xt[:, s, :], in_=x_all[6144 + ti * 128:6144 + (ti + 1) * 128, :]), writes=[xk])
                        for hf in range(2):
                            bi = 4 + hf
                            bk, bkey = bank(bi)
                            for k in range(8):
                                P.op(PE, lambda e, k=k, hf=hf, bk=bk, j=j: e.matmul(bk[:], mgT[:, k, j * 128:(j + 1) * 128], wo[hf][0][:, k, :], start=(k == 0), stop=(k == 7)),
                                     reads=mgk + [wo[hf][1]], writes=[bkey])
                            P.op(DVE, lambda e, hf=hf, bk=bk, s=s: e.tensor_tensor(out=xt[:, s, hf * 512:(hf + 1) * 512], in0=bk[:], in1=xt[:, s, hf * 512:(hf + 1) * 512], op=ALU.add),
                                 reads=[bkey, xk], writes=[xk])
                        for hf in range(2):
                            P.dma(ACT, lambda e, ti=ti, s=s, hf=hf: e.dma_start(out=X1[ti * 128:(ti + 1) * 128, hf * 512:(hf + 1) * 512], in_=xt[:, s, hf * 512:(hf + 1) * 512]), reads=[xk], writes=[("X1", ti, hf)])
                        if stop <= 2.9 and tg == DBG_TG:
                            st.close()
                            raise _Stop
                        norm_T((xt, xn, junk, ss, rs), None, ti, gmoef, "gmoe", h2g, "hT", j * 128, "b_")
                        if stop <= 2.95 and tg == DBG_TG:
                            st.close()
                            raise _Stop
                        bk, bkey = bank(6)
                        for k in range(8):
                            P.op(PE, lambda e, k=k, bk=bk, j=j: e.matmul(bk[:, 0:36], h2g[:, k, j * 128:(j + 1) * 128].bitcast(F32), wrt[:, k, :], start=(k == 0), stop=(k == 7)),
                                 reads=["hT", "wrt"], writes=[bkey])
                        R = lambda nm: rt[nm]
                        D = lambda fn, rd, wr_: P.op(DVE, fn, reads=rd, writes=wr_)
                        D(lambda e, bk=bk: e.tensor_tensor(out=lg[:], in0=bk[:, 0:36], in1=brt[:], op=ALU.add), [bkey, "brt"], ["lg"])
                        if stop <= 2.96 and tg == DBG_TG:
                            st.close()
                            raise _Stop
                        D(lambda e: e.tensor_reduce(out=R("a")[:, 0:1], in_=lg[:, 0:4], axis=AX.X, op=ALU.max), ["lg"], ["ra"])
                        D(lambda e: e.tensor_scalar(out=R("b")[:, 0:4], in0=lg[:, 0:4], scalar1=R("a")[:, 0:1], scalar2=None, op0=ALU.subtract), ["lg", "ra"], ["rb"])
                        P.op(ACT, lambda e: e.activation(out=R("b")[:, 0:4], in_=R("b")[:, 0:4], func=AF.Exp, accum_out=R("a")[:, 1:2]), reads=["rb"], writes=["rb", "ra2"])
                        D(lambda e: e.reciprocal(out=R("a")[:, 2:3], in_=R("a")[:, 1:2]), ["ra2"], ["gp"])
                        D(lambda e: e.tensor_scalar(out=R("c")[:, 0:4], in0=lg[:, 0:4], scalar1=R("a")[:, 0:1], scalar2=None, op0=ALU.is_ge), ["lg", "ra"], ["rc"])
                        if stop <= 2.97 and tg == DBG_TG:
                            st.close()
                            raise _Stop
                        D(lambda e: e.tensor_scalar(out=R("d")[:, 0:4], in0=R("c")[:, 0:4], scalar1=-1.0, scalar2=1.0e4, op0=ALU.add, op1=ALU.mult), ["rc"], ["rd"])
                        lg3 = lg[:, 4:36].rearrange("p (g j) -> p g j", g=4)
                        e3 = R("e")[:, 0:32].rearrange("p (g j) -> p g j", g=4)
                        for g_ in range(4):
                            D(lambda e, g_=g_: e.tensor_scalar(out=e3[:, g_, :], in0=lg3[:, g_, :], scalar1=R("d")[:, g_:g_ + 1], scalar2=None, op0=ALU.add), ["lg", "rd"], ["re"])
                        D(lambda e: e.tensor_reduce(out=R("f")[:, 0:1], in_=R("e")[:, 0:32], axis=AX.X, op=ALU.max), ["re"], ["m1"])
                        D(lambda e: e.tensor_scalar(out=R("g")[:, 0:32], in0=R("e")[:, 0:32], scalar1=R("f")[:, 0:1], scalar2=None, op0=ALU.is_ge), ["re", "m1"], ["oh1"])
                        D(lambda e: e.scalar_tensor_tensor(out=R("h")[:, 0:32], in0=R("g")[:, 0:32], scalar=-1.0e4, in1=R("e")[:, 0:32], op0=ALU.mult, op1=ALU.add), ["oh1", "re"], ["rh"])
                        D(lambda e: e.tensor_reduce(out=R("f")[:, 1:2], in_=R("h")[:, 0:32], axis=AX.X, op=ALU.max), ["rh"], ["m2"])
                        D(lambda e: e.tensor_scalar(out=R("h")[:, 0:32], in0=R("h")[:, 0:32], scalar1=R("f")[:, 1:2], scalar2=None, op0=ALU.is_ge), ["rh", "m2"], ["oh2"])
                        D(lambda e: e.tensor_tensor(out=R("f")[:, 2:3], in0=R("f")[:, 1:2], in1=R("f")[:, 0:1], op=ALU.subtract), ["m1", "m2"], ["df"])
                        P.op(ACT, lambda e: e.activation(out=R("f")[:, 2:3], in_=R("f")[:, 2:3], func=AF.Exp), reads=["df"], writes=["df"])
                        D(lambda e: e.tensor_scalar(out=R("f")[:, 2:3], in0=R("f")[:, 2:3], scalar1=1.0, scalar2=None, op0=ALU.add), ["df"], ["df"])
                        D(lambda e: e.reciprocal(out=R("f")[:, 3:4], in_=R("f")[:, 2:3]), ["df"], ["w1"])
                        D(lambda e: e.tensor_tensor(out=R("f")[:, 3:4], in0=R("f")[:, 3:4], in1=R("a")[:, 2:3], op=ALU.mult), ["w1", "gp"], ["w1"])
                        D(lambda e: e.tensor_tensor(out=R("f")[:, 4:5], in0=R("a")[:, 2:3], in1=R("f")[:, 3:4], op=ALU.subtract), ["w1", "gp"], ["w2"])
                        D(lambda e, ti=ti: e.tensor_copy(out=wts[:, ti, 0:1], in_=R("f")[:, 3:4]), ["w1"], [("wts", ti)])
                        D(lambda e, ti=ti: e.tensor_copy(out=wts[:, ti, 1:2], in_=R("f")[:, 4:5]), ["w2", ("wts", ti)], [("wts", ti)])
                        D(lambda e: e.tensor_tensor(out=Am[:], in0=R("g")[:, 0:32], in1=R("h")[:, 0:32], op=ALU.add), ["oh1", "oh2"], ["Am"])
                        bkp, bkpkey = bank(7)
                        P.op(PE, lambda e, bkp=bkp: e.matmul(bkp[:, 0:32], ustr[:], Am[:], start=True, stop=True), reads=["ustr", "Am"], writes=[bkpkey])
                        P.op(PE, lambda e, bkp=bkp: e.matmul(bkp[:, 32:64], ones_t[:], Am[:], start=True, stop=True), reads=["ones_t", "Am"], writes=[bkpkey])
                        D(lambda e, bkp=bkp: e.tensor_tensor(out=posf[:], in0=bkp[:, 0:32], in1=cntb[:], op=ALU.add), [bkpkey, "cntb"], ["posf"])
                        D(lambda e, bkp=bkp: e.tensor_tensor(out=cntb[:], in0=bkp[:, 32:64], in1=cntb[:], op=ALU.add), [bkpkey, "cntb", "posf"], ["cntb"])
                        for kk, ohn in ((0, "g"), (1, "h")):
                            okey = "oh1" if kk == 0 else "oh2"
                            D(lambda e, ohn=ohn: e.tensor_tensor(out=ptmp[:], in0=R(ohn)[:, 0:32], in1=posf[:], op=ALU.mult), [okey, "posf"], ["ptmp"])
                            D(lambda e, kk=kk: e.tensor_reduce(out=idxf[:, 2 + kk:3 + kk], in_=ptmp[:], axis=AX.X, op=ALU.add), ["ptmp"], [("idxf", kk)])
                            D(lambda e, ohn=ohn: e.tensor_tensor(out=ptmp[:], in0=R(ohn)[:, 0:32], in1=ecap[:], op=ALU.mult), [okey, "ecap", ("idxf", kk)], ["ptmp"])
                            D(lambda e, kk=kk: e.tensor_reduce(out=idxf[:, kk:kk + 1], in_=ptmp[:], axis=AX.X, op=ALU.add), ["ptmp"], [("idxf", kk)])
                            D(lambda e, kk=kk: e.tensor_tensor(out=idxf[:, kk:kk + 1], in0=idxf[:, kk:kk + 1], in1=idxf[:, 2 + kk:3 + kk], op=ALU.add), [("idxf", kk)], [("idxf", kk)])
                            D(lambda e, kk=kk: e.tensor_scalar(out=idxf[:, 2 + kk:3 + kk], in0=idxf[:, 2 + kk:3 + kk], scalar1=float(CAP) - 0.5, scalar2=1.0e6, op0=ALU.is_gt, op1=ALU.mult), [("idxf", kk)], [("idxf", kk)])
                            D(lambda e, kk=kk: e.tensor_tensor(out=idxf[:, kk:kk + 1], in0=idxf[:, kk:kk + 1], in1=idxf[:, 2 + kk:3 + kk], op=ALU.add), [("idxf", kk)], [("idxf", kk)])
                            D(lambda e, kk=kk, ti=ti: e.tensor_copy(out=idxi[:, ti * 2 + kk:ti * 2 + kk + 1], in_=idxf[:, kk:kk + 1]), [("idxf", kk)], [("idxi", ti, kk)])
                            P.dma(POOL, lambda e, kk=kk, ti=ti: e.indirect_dma_start(out=Xscr[:, :], out_offset=bass.IndirectOffsetOnAxis(ap=idxi[:, ti * 2 + kk:ti * 2 + kk + 1], axis=0),
                                                                                     in_=xn[:, ti % 2, :], in_offset=None, bounds_check=NROW - 1, oob_is_err=False),
                                  reads=[("idxi", ti, kk), ("b_xn", ti % 2)] + [("Xz", r_) for r_ in range(0, 64, 2)], writes=[("Xs", ti, kk)])
                        if stop <= 2.98 and tg == DBG_TG:
                            st.close()
                            raise _Stop
                    if stop <= 2.99 and tg == DBG_TG:
                        st.close()
                        raise _Stop
                    if stop <= 2.995 and tg == DBG_TG:
                        st.close()
                        raise _Stop
        except _Stop:
            pass
        P.barrier()
        if debug and stop == 3:
            dX1 = nc.dram_tensor("dX1", [NOWN, 1024], F32, kind="ExternalOutput").ap()
            dIdx = nc.dram_tensor("dIdx", [128, 32], mybir.dt.int32, kind="ExternalOutput").ap()
            dWD = nc.dram_tensor("dWD", [128, 16, 2], F32, kind="ExternalOutput").ap()
            dXs = nc.dram_tensor("dXs", [NROW, 1024], F32, kind="ExternalOutput").ap()
            for i_ in range(4):
                P.dma(SP, lambda e, i_=i_: e.dma_start(out=dX1[i_ * 512:(i_ + 1) * 512, :], in_=X1[i_ * 512:(i_ + 1) * 512, :]), writes=[("dX1", i_)])
            for i_ in range(8):
                P.dma(SP, lambda e, i_=i_: e.dma_start(out=dXs[i_ * 1024:(i_ + 1) * 1024, :], in_=Xscr[i_ * 1024:(i_ + 1) * 1024, :]), writes=[("dXs", i_)])
            P.dma(SP, lambda e: e.dma_start(out=dWD, in_=wts[:]), writes=["dWD"])
            P.dma(SP, lambda e: e.dma_start(out=dIdx, in_=idxi[:]), writes=["dIdx"])
            P.barrier()
        if stop <= 3:
            wdense_cm.close(); P.emit(); return nc

        with contextlib.ExitStack() as st:
            gmoef = make_gfull(st, "gmoe4", gmoe)
            wg = [sb(st, "wg%d" % i, [128, 8, 512], F32R) for i in range(2)]
            wu = [sb(st, "wu%d" % i, [128, 8, 512], F32R) for i in range(2)]
            wd = [sb(st, "wd%d" % i, [128, 4, 1024], F32R) for i in range(2)]
            Xr = sb(st, "Xr", [128, 2, 2, 1024]); XeT = sb(st, "XeT", [128, 2, 8, CAP], F32R)
            aTm = sb(st, "aTm", [128, 2, 4, CAP], F32R); sil = sb(st, "sil", [128, 2, CAP])
            Yt = sb(st, "Yt", [128, 2, 2, 1024])
            gf = sb(st, "gf", [128, 1024]); xo = sb(st, "xo", [128, 2, 1024]); ygl = [[sb(st, "yg%d%d" % (a_, b_), [128, 1024]) for b_ in range(2)] for a_ in range(2)]
            fss = sb(st, "fss", [128, 16])
            P.dma(SP, lambda e: e.dma_start(out=gf[:], in_=gfin), writes=["gf"])
            allXs = [("Xs", ti, kk) for ti in range(16) for kk in range(2)]
            for ex in range(32):
                s = ex % 2
                wload(wg[s][:], weg[ex].rearrange("(k p) c -> p k c", p=128), ("wg", s))
                wload(wu[s][:], weu[ex].rearrange("(k p) c -> p k c", p=128), ("wu", s))
                wload(wd[s][:], wed[ex].rearrange("(k p) c -> p k c", p=128), ("wd", s))
                P.dma(SP, lambda e, ex=ex, s=s: e.dma_start(out=Xr[:, s], in_=Xscr[ex * CAP:(ex + 1) * CAP, :].rearrange("(rt p) c -> p rt c", p=128)),
                      reads=allXs, writes=[("Xr", s)])
                for rt in range(2):
                    for hh in range(2):
                        bi = pcnt[0] % 2; pcnt[0] += 1
                        bk, bkey = bank(bi)
                        for j in range(4):
                            k = hh * 4 + j
                            P.op(PE, lambda e, k=k, j=j, bk=bk, rt=rt, s=s: e.transpose(out=bk[:, j * 128:(j + 1) * 128], in_=Xr[:, s, rt, k * 128:(k + 1) * 128], identity=ident[:]),
                                 reads=[("Xr", s), "ident"], writes=[bkey])
                        P.op(DVE, lambda e, hh=hh, bk=bk, rt=rt, s=s: e.tensor_tensor(out=XeT[:, s, hh * 4:hh * 4 + 4, rt * 128:(rt + 1) * 128],
                                                                                   in0=bk[:].rearrange("p (k t) -> p k t", k=4), in1=gmoef[:, hh * 4:hh * 4 + 4, :], op=ALU.mult),
                             reads=[bkey, "gmoe4"], writes=[("XeT", s, rt, hh)])
                xek = [("XeT", s, rt, hh) for rt in range(2) for hh in range(2)]
                for fc in range(4):
                    pr = 2 + (pcnt[0] % 2) * 2; pcnt[0] += 1
                    (bg_, bgk), (bu_, buk) = bank(pr), bank(pr + 1)
                    for k in range(8):
                        P.op(PE, lambda e, k=k, fc=fc, bg_=bg_, s=s: e.matmul(bg_[:, 0:CAP], wg[s][:, k, fc * 128:(fc + 1) * 128], XeT[:, s, k, :], start=(k == 0), stop=(k == 7)),
                             reads=[("wg", s)] + xek, writes=[bgk])
                    for k in range(8):
                        P.op(PE, lambda e, k=k, fc=fc, bu_=bu_, s=s: e.matmul(bu_[:, 0:CAP], wu[s][:, k, fc * 128:(fc + 1) * 128], XeT[:, s, k, :], start=(k == 0), stop=(k == 7)),
                             reads=[("wu", s)] + xek, writes=[buk])
                    ss_ = fc % 2
                    P.op(ACT, lambda e, bg_=bg_, ss_=ss_: e.activation(out=sil[:, ss_, :], in_=bg_[:, 0:CAP], func=AF.Silu), reads=[bgk], writes=[("sil", ss_)])
                    P.op(DVE, lambda e, bu_=bu_, ss_=ss_, fc=fc, s=s: e.tensor_tensor(out=aTm[:, s, fc, :], in0=bu_[:, 0:CAP], in1=sil[:, ss_, :], op=ALU.mult),
                         reads=[buk, ("sil", ss_)], writes=[("aTm", s, fc)])
                for rt in range(2):
                    for hf in range(2):
                        bi = 6 + (pcnt[0] % 2); pcnt[0] += 1
                        bk, bkey = bank(bi)
                        for k in range(4):
                            P.op(PE, lambda e, k=k, bk=bk, rt=rt, hf=hf, s=s: e.matmul(bk[:], aTm[:, s, k, rt * 128:(rt + 1) * 128], wd[s][:, k, hf * 512:(hf + 1) * 512], start=(k == 0), stop=(k == 3)),
                                 reads=[("aTm", s, k_) for k_ in range(4)] + [("wd", s)], writes=[bkey])
                        P.op(ACT, lambda e, bk=bk, rt=rt, hf=hf, s=s: e.copy(out=Yt[:, s, rt, hf * 512:(hf + 1) * 512], in_=bk[:]), reads=[bkey], writes=[("Yt", s, rt, hf)])
                P.dma(ACT, lambda e, ex=ex, s=s: e.dma_start(out=Yscr[ex * CAP:(ex + 1) * CAP, :].rearrange("(rt p) c -> p rt c", p=128), in_=Yt[:, s]),
                      reads=[("Yt", s, rt, hf) for rt in range(2) for hf in range(2)], writes=[("Ys", ex)])
            allYs = [("Ys", ex) for ex in range(32)]
            for ti in range(16):
                s = ti % 2
                xk = ("xo", s)
                P.dma(SP, lambda e, ti=ti, s=s: e.dma_start(out=xo[:, s, :], in_=X1[ti * 128:(ti + 1) * 128, :]), reads=[("X1", ti, 0), ("X1", ti, 1)], writes=[xk])
                for kk in range(2):
                    P.op(DVE, lambda e, s=s, kk=kk: e.memset(ygl[s][kk][:, :], 0.0), writes=[("yg", s, kk)])
                    P.dma(POOL, lambda e, ti=ti, s=s, kk=kk: e.indirect_dma_start(out=ygl[s][kk][:, :], out_offset=None, in_=Yscr[:, :],
                                                                                 in_offset=bass.IndirectOffsetOnAxis(ap=idxi[:, ti * 2 + kk:ti * 2 + kk + 1], axis=0), bounds_check=NROW - 1, oob_is_err=False),
                          reads=allYs + [("idxi", ti, kk), ("yg", s, kk)], writes=[("yg", s, kk)])
                    P.op(DVE, lambda e, s=s, kk=kk, ti=ti: e.scalar_tensor_tensor(out=xo[:, s, :], in0=ygl[s][kk][:, :], scalar=wts[:, ti, kk:kk + 1], in1=xo[:, s, :], op0=ALU.mult, op1=ALU.add),
                         reads=[("yg", s, kk), ("wts", ti), xk], writes=[xk])
                P.op(ACT, lambda e, s=s, ti=ti: e.activation(out=ygl[s][0][:, :], in_=xo[:, s, :], func=AF.Square, accum_out=fss[:, ti:ti + 1]), reads=[xk, ("yg", s, 0)], writes=[("yg", s, 0), ("fss", ti)])
                P.op(DVE, lambda e, ti=ti: e.tensor_scalar(out=fss[:, ti:ti + 1], in0=fss[:, ti:ti + 1], scalar1=1.0 / 1024, scalar2=EPS, op0=ALU.mult, op1=ALU.add), reads=[("fss", ti)], writes=[("fss", ti)])
                P.op(ACT, lambda e, ti=ti: e.activation(out=fss[:, ti:ti + 1], in_=fss[:, ti:ti + 1], func=AF.Sqrt), reads=[("fss", ti)], writes=[("fss", ti)])
                P.op(DVE, lambda e, ti=ti: e.reciprocal(out=fss[:, ti:ti + 1], in_=fss[:, ti:ti + 1]), reads=[("fss", ti)], writes=[("fss", ti)])
                P.op(DVE, lambda e, s=s, ti=ti: e.scalar_tensor_tensor(out=xo[:, s, :], in0=xo[:, s, :], scalar=fss[:, ti:ti + 1], in1=gf[:], op0=ALU.mult, op1=ALU.mult),
                     reads=[xk, ("fss", ti), "gf"], writes=[xk])
                P.dma(ACT, lambda e, ti=ti, s=s: e.dma_start(out=y_out[ti * 128:(ti + 1) * 128, :], in_=xo[:, s, :]), reads=[xk], writes=[("yout", ti)])
        P.barrier()
        wdense_cm.close()
        P.emit()
        global LASTP
        LASTP = P
    return nc


def _prep(inp):
    f = np.float32
    x = np.asarray(inp["x"], f)
    w_in = np.asarray(inp["w_in"], f)[0]
    wl = np.zeros((1024, WCOLS), f)
    wl[:, 0:512] = w_in[:, 0:512]
    k0, k1 = w_in[:, 512:576], w_in[:, 576:640]
    zc = np.zeros((1024, 64), f)
    wl[:, CK:CK + 512] = np.concatenate([k0, zc, zc, k0, k1, zc, zc, k1], 1)
    wl[:, CV:CV + 128] = w_in[:, 640:768]
    up = np.zeros((1024, 32, 32), f); up[:, :, :16] = w_in[:, 768:1280].reshape(1024, 32, 16)
    wl[:, CU:CU + 1024] = up.reshape(1024, 1024)
    wl[:, CG:] = w_in[:, 1280:]

    def pad_rows(w):
        o = np.zeros((32, 32, w.shape[1]), f); o[:, :16] = w.reshape(32, 16, -1); return o.reshape(1024, -1)

    def pad_vec(v):
        o = np.zeros((32, 32), f); o[:, :16] = v.reshape(32, 16); return o.reshape(1024)

    wglu = pad_rows(np.asarray(inp["w_glu"], f)[0])
    wglu = pad_rows(wglu.T.copy()).T.copy()
    bglu = pad_vec(np.asarray(inp["b_glu"], f)[0]).reshape(8, 128).T.copy()
    wsb = pad_rows(np.asarray(inp["w_ssm_branch"], f)[0])
    a_re = np.asarray(inp["ssm_a_re"], f)[0]; a_im = np.asarray(inp["ssm_a_im"], f)[0]; ldt = np.asarray(inp["ssm_log_dt"], f)[0]
    b_re = np.asarray(inp["ssm_b_re"], f)[0]; b_im = np.asarray(inp["ssm_b_im"], f)[0]
    c_re = np.asarray(inp["ssm_c_re"], f)[0]; c_im = np.asarray(inp["ssm_c_im"], f)[0]
    dup = lambda a: np.concatenate([a.T, a.T], 0).copy()
    are1, aim1 = dup(a_re), dup(a_im)
    ldt1 = np.broadcast_to(ldt[None, :], (128, 32)).copy()

    def lay2(a_gp):
        o = a_gp.reshape(8, 4, 64)
        o = np.broadcast_to(o[:, :, None, :], (8, 4, 32, 64))
        return o.transpose(1, 2, 0, 3).reshape(128, 512).copy()

    are2, aim2 = lay2(a_re), lay2(a_im)
    ldt2 = lay2(np.broadcast_to(ldt[:, None], (32, 64)))

    def lay2b(b):
        o = np.zeros((8, 4, 32, 64), f)
        o[:, :, :16, :] = b.reshape(8, 4, 64, 16).transpose(0, 1, 3, 2)
        return o.transpose(1, 2, 0, 3).reshape(128, 512).copy()

    bre2, bim2 = lay2b(b_re), lay2b(b_im)
    cst = np.concatenate([c_re.transpose(2, 0, 1), c_im.transpose(2, 0, 1)], 0).reshape(128, 512).copy()
    dpad = pad_vec(np.asarray(inp["ssm_d"], f)[0]).reshape(8, 128).T.copy()
    fm = lambda v, n: np.asarray(v, f).reshape(n, 128).T.copy()
    ident = np.eye(128, dtype=f)
    jswap = np.zeros((128, 128), f); jswap[np.arange(128), (np.arange(128) + 64) % 128] = 1
    sgn = np.ones((128, 1), f); sgn[64:] = -1
    s_ = np.arange(128)[:, None]; q_ = np.arange(128)[None, :]
    mcur = np.where(q_ >= s_, 0.0, NEG).astype(f); mprev = np.where(s_ > q_, 0.0, NEG).astype(f)
    t4 = lambda m: np.tile(m, (1, 4)).copy()
    wr = np.concatenate([np.asarray(inp["w_router_group"], f)[0], np.asarray(inp["w_router_expert"], f)[0]], 1)
    wr = wr.reshape(8, 128, 36).transpose(1, 0, 2).copy()
    brr = np.concatenate([np.asarray(inp["b_router_group"], f)[0], np.asarray(inp["b_router_expert"], f)[0]])
    common = dict(
        w_in=wl, gmix=fm(inp["norm_mix"][0], 8), gmoe=fm(inp["norm_moe"][0], 8),
        gfin=np.broadcast_to(np.asarray(inp["norm_final"], f)[None, :], (128, 1024)).copy(),
        bgate=fm(inp["b_gate"][0], 16), sinks=np.broadcast_to(np.asarray(inp["attn_sinks"], f)[0][None, :], (128, 8)).copy(),
        ident=ident, jswap=jswap, sgn=sgn, ustr=np.triu(np.ones((128, 128), f), 1), ones=np.ones((128, 128), f), ecap=np.broadcast_to((np.arange(32, dtype=f) * CAP)[None, :], (128, 32)).copy(), m3=(np.arange(128) >= 96).astype(f).reshape(128, 1), mcur=t4(mcur), mprev=t4(mprev),
        are1=are1, aim1=aim1, ldt1=ldt1, are2=are2, aim2=aim2, ldt2=ldt2, bre2=bre2, bim2=bim2, cst=cst, dpad=dpad,
        wglu=wglu, bglu=bglu, wab=np.asarray(inp["w_attn_branch"], f)[0], wsb=wsb, wout=np.asarray(inp["w_out"], f)[0],
        wr=wr, br=np.broadcast_to(brr[None, :], (128, 36)).copy(),
        weg=np.asarray(inp["w_expert_gate"], f)[0], weu=np.asarray(inp["w_expert_up"], f)[0], wed=np.asarray(inp["w_expert_down"], f)[0],
    )
    maps = []
    for c in range(8):
        b, seg = c // 4, c % 4
        xa = np.zeros((NALL, 1024), f)
        end = (seg + 1) * 2048
        xa[NALL - end:] = x[b, :end]
        m = dict(common)
        m["x_all"] = xa
        m["mprev0"] = t4(np.full((128, 128), NEG, f)) if seg == 0 else t4(mprev)
        maps.append(m)
    return maps


def kernel(**inputs):
    maps = _prep(inputs)
    nc = build_nc()
    res = run_bass_kernel_spmd(nc, maps, core_ids=list(range(8)))
    out = np.zeros((2, 8192, 1024), np.float32)
    for c in range(8):
        b, seg = c // 4, c % 4
        out[b, seg * 2048:(seg + 1) * 2048] = res.results[c]["y_out"]
    return out
```
